# Optimizing a Trainium2 kernel written in Bass

```python
import math
import jax
import jax.numpy as jnp
from jax import lax
import numpy as np

D_MODEL = 1024
BATCH = 8
SEQ = 2048
DEPTH = 4

SSM_WIDTH = D_MODEL // 2
SSM_GROUP = 16
SSM_GROUPS = SSM_WIDTH // SSM_GROUP
SSM_STATE = 64
DT_MIN = 0.001
DT_MAX = 0.1
DIFF_HEADS = 4
DIFF_HEAD_DIM = 64
DIFF_V_DIM = 2 * DIFF_HEAD_DIM
DIFF_WIDTH = DIFF_HEADS * DIFF_V_DIM
FOX_HEADS = 8
FOX_HEAD_DIM = 64
FOX_WIDTH = FOX_HEADS * FOX_HEAD_DIM
N_BRANCH = 3
BRANCH_WIDTH = SSM_WIDTH
Q_BLOCK = 128
ROPE_THETA = 10000.0
N_EXPERT_GROUPS = 4
EXPERTS_PER_GROUP = 4
N_EXPERTS = N_EXPERT_GROUPS * EXPERTS_PER_GROUP
D_EXPERT = 256
TOP_K = 2
ALPHA = (2 * DEPTH) ** 0.25
BETA = (8 * DEPTH) ** -0.25
LN_EPS = 1e-5
RMS_EPS = 1e-6
NEG_INF = -1e30

C_SSM = SSM_WIDTH
C_DQ = C_SSM + 2 * DIFF_HEADS * DIFF_HEAD_DIM
C_DK = C_DQ + 2 * DIFF_HEADS * DIFF_HEAD_DIM
C_DV = C_DK + DIFF_WIDTH
C_FQ = C_DV + FOX_WIDTH
C_FK = C_FQ + FOX_WIDTH
C_FV = C_FK + FOX_WIDTH
C_FF = C_FV + FOX_HEADS
D_IN = C_FF + N_BRANCH * D_MODEL
SPLIT_IDX = [C_SSM, C_DQ, C_DK, C_DV, C_FQ, C_FK, C_FV, C_FF]

kernel_name = 'hybrid_s5_diffattn_fox_hiermoe_deepnorm'


def layer_norm(x, g, b):
    x32 = x.astype(jnp.float32)
    mu = jnp.mean(x32, axis=-1, keepdims=True)
    var = jnp.mean(jnp.square(x32 - mu), axis=-1, keepdims=True)
    return ((x32 - mu) * lax.rsqrt(var + LN_EPS) * g.astype(jnp.float32) + b.astype(jnp.float32)).astype(x.dtype)


def apply_rope(t, cos, sin):
    t1, t2 = jnp.split(t, 2, axis=-1)
    return t * cos + jnp.concatenate([-t2, t1], axis=-1) * sin


def to_blocks(t):
    b, s = t.shape[0], t.shape[1]
    t = t.reshape((b, s // Q_BLOCK, Q_BLOCK) + t.shape[2:])
    return jnp.moveaxis(t, 1, 0)


def from_blocks(t):
    nb, b, q = t.shape[0], t.shape[1], t.shape[2]
    return jnp.moveaxis(t, 0, 1).reshape((b, nb * q) + t.shape[3:])


def ssm_combine(e1, e2):
    a1r, a1i, b1r, b1i = e1
    a2r, a2i, b2r, b2i = e2
    ar = a2r * a1r - a2i * a1i
    ai = a2r * a1i + a2i * a1r
    br = a2r * b1r - a2i * b1i + b2r
    bi = a2r * b1i + a2i * b1r + b2i
    return (ar, ai, br, bi)


def s5_branch(u, lam_re, lam_im, log_dt, b_re, b_im, c_re, c_im, d, w_glu):
    f32 = jnp.float32
    bsz, s, _ = u.shape
    u = u.astype(f32).reshape(bsz, s, SSM_GROUPS, SSM_GROUP)
    lr = lam_re.astype(f32)
    li = lam_im.astype(f32)
    dt = jnp.exp(log_dt.astype(f32))[:, None]
    mag = jnp.exp(lr * dt)
    ang = li * dt
    ar = mag * jnp.cos(ang)
    ai = mag * jnp.sin(ang)
    er = ar - 1.0
    ei = ai
    den = lr * lr + li * li
    qr = (er * lr + ei * li) / den
    qi = (ei * lr - er * li) / den
    br = b_re.astype(f32)
    bi = b_im.astype(f32)
    bbr = qr[:, :, None] * br - qi[:, :, None] * bi
    bbi = qr[:, :, None] * bi + qi[:, :, None] * br
    bur = jnp.einsum('bsgn,gpn->bsgp', u, bbr)
    bui = jnp.einsum('bsgn,gpn->bsgp', u, bbi)
    shape = bur.shape
    elems = (jnp.broadcast_to(ar, shape), jnp.broadcast_to(ai, shape), bur, bui)
    _, _, xr, xi = lax.associative_scan(ssm_combine, elems, axis=1)
    y = (jnp.einsum('gnp,bsgp->bsgn', c_re.astype(f32), xr)
         - jnp.einsum('gnp,bsgp->bsgn', c_im.astype(f32), xi)
         + d.astype(f32).reshape(SSM_GROUPS, SSM_GROUP) * u)
    y = jax.nn.gelu(y.reshape(bsz, s, SSM_WIDTH))
    val, gate = jnp.split(y @ w_glu.astype(f32), 2, axis=-1)
    return val * jax.nn.sigmoid(gate)


def diff_attention(q, k, v, lam, lam_init, norm_g, cos, sin):
    f32 = jnp.float32
    bsz, s, _ = q.shape
    q = q.reshape(bsz, s, DIFF_HEADS, 2, DIFF_HEAD_DIM)
    k = k.reshape(bsz, s, DIFF_HEADS, 2, DIFF_HEAD_DIM)
    v = v.reshape(bsz, s, DIFF_HEADS, DIFF_V_DIM)
    cs = cos[None, :, None, None, :].astype(q.dtype)
    sn = sin[None, :, None, None, :].astype(q.dtype)
    q = apply_rope(q, cs, sn)
    k = apply_rope(k, cs, sn)
    lam32 = lam.astype(f32)
    lam_full = (jnp.exp(jnp.sum(lam32[0] * lam32[1]))
                - jnp.exp(jnp.sum(lam32[2] * lam32[3])) + lam_init)
    scale = DIFF_HEAD_DIM ** -0.5
    kpos = jnp.arange(s)

    def block(args):
        qb, i = args
        sc = jnp.einsum('bqhcd,bkhcd->bhcqk', qb, k, preferred_element_type=f32) * scale
        qpos = i * Q_BLOCK + jnp.arange(Q_BLOCK)
        mask = kpos[None, :] <= qpos[:, None]
        p = jax.nn.softmax(jnp.where(mask, sc, NEG_INF), axis=-1)
        a = p[:, :, 0] - lam_full * p[:, :, 1]
        return jnp.einsum('bhqk,bkhe->bqhe', a.astype(v.dtype), v)

    o = from_blocks(lax.map(block, (to_blocks(q), jnp.arange(s // Q_BLOCK))))
    o32 = o.astype(f32)
    o32 = o32 * lax.rsqrt(jnp.mean(jnp.square(o32), axis=-1, keepdims=True) + RMS_EPS)
    o32 = o32 * norm_g.astype(f32).reshape(DIFF_HEADS, DIFF_V_DIM) * (1.0 - lam_init)
    return o32.reshape(bsz, s, DIFF_WIDTH)


def forgetting_attention(q, k, v, f_logit, f_bias):
    f32 = jnp.float32
    bsz, s, _ = q.shape
    q = q.reshape(bsz, s, FOX_HEADS, FOX_HEAD_DIM)
    k = k.reshape(bsz, s, FOX_HEADS, FOX_HEAD_DIM)
    v = v.reshape(bsz, s, FOX_HEADS, FOX_HEAD_DIM)
    logf = jax.nn.log_sigmoid(f_logit.astype(f32) + f_bias.astype(f32))
    c = jnp.cumsum(logf, axis=1)
    c_k = jnp.transpose(c, (0, 2, 1))
    scale = FOX_HEAD_DIM ** -0.5
    kpos = jnp.arange(s)

    def block(args):
        qb, cb, i = args
        sc = jnp.einsum('bqhd,bkhd->bhqk', qb, k, preferred_element_type=f32) * scale
        sc = sc + jnp.transpose(cb, (0, 2, 1))[:, :, :, None] - c_k[:, :, None, :]
        qpos = i * Q_BLOCK + jnp.arange(Q_BLOCK)
        mask = kpos[None, :] <= qpos[:, None]
        p = jax.nn.softmax(jnp.where(mask, sc, NEG_INF), axis=-1)
        return jnp.einsum('bhqk,bkhd->bqhd', p.astype(v.dtype), v)

    o = from_blocks(lax.map(block, (to_blocks(q), to_blocks(c), jnp.arange(s // Q_BLOCK))))
    return o.reshape(bsz, s, FOX_WIDTH)


def token_mixer(h, w_in, w_branch, w_out, lam_re, lam_im, log_dt, b_re, b_im, c_re, c_im, d,
                w_glu, diff_lam, diff_g, fox_b, lam_init, cos, sin):
    bsz, s, _ = h.shape
    proj = h @ w_in
    u, dq, dk, dv, fq, fk, fv, ff, gl = jnp.split(proj, SPLIT_IDX, axis=-1)
    y_ssm = s5_branch(u, lam_re, lam_im, log_dt, b_re, b_im, c_re, c_im, d, w_glu)
    y_diff = diff_attention(dq, dk, dv, diff_lam, lam_init, diff_g, cos, sin)
    y_fox = forgetting_attention(fq, fk, fv, ff, fox_b)
    branches = jnp.stack([y_ssm, y_diff, y_fox.astype(y_ssm.dtype)], axis=2)
    proj_b = jnp.einsum('bsnw,nwd->bsnd', branches, w_branch)
    gates = jax.nn.sigmoid(gl.reshape(bsz, s, N_BRANCH, D_MODEL).astype(jnp.float32))
    merged = jnp.sum(gates * proj_b, axis=2)
    return (merged @ w_out).astype(h.dtype)


def hier_moe(x, w_g, b_g, w_e, b_e, w_gate, w_up, w_down):
    f32 = jnp.float32
    bsz, s, dm = x.shape
    t = x.reshape(-1, dm)
    pg = jax.nn.softmax((t @ w_g).astype(f32) + b_g.astype(f32), axis=-1)
    gp, gi = lax.top_k(pg, 1)
    el = ((t @ w_e).astype(f32) + b_e.astype(f32)).reshape(-1, N_EXPERT_GROUPS, EXPERTS_PER_GROUP)
    el_sel = jnp.take_along_axis(el, gi[:, :, None], axis=1)[:, 0]
    ev, ei = lax.top_k(el_sel, TOP_K)
    w = gp * jax.nn.softmax(ev, axis=-1)
    eidx = gi * EXPERTS_PER_GROUP + ei
    gate = jnp.sum(jax.nn.one_hot(eidx, N_EXPERTS, dtype=f32) * w[..., None], axis=1)
    hg = jnp.einsum('td,edf->tef', t, w_gate)
    hu = jnp.einsum('td,edf->tef', t, w_up)
    hidden = jax.nn.silu(hg) * hu * gate[:, :, None].astype(hg.dtype)
    out = jnp.einsum('tef,efd->td', hidden, w_down)
    return out.reshape(bsz, s, dm).astype(x.dtype)


def setup_inputs(seed: int = 0) -> dict:
    key = jax.random.key(seed)
    ks = jax.random.split(key, 32)
    f32 = jnp.float32

    def nrm(k, shape, scale):
        return jax.random.normal(k, shape, f32) * scale

    G, P = SSM_GROUPS, SSM_STATE
    x = nrm(ks[0], (BATCH, SEQ, D_MODEL), 1.0)
    w_in = nrm(ks[1], (DEPTH, D_MODEL, D_IN), D_MODEL ** -0.5)
    w_branch = nrm(ks[2], (DEPTH, N_BRANCH, BRANCH_WIDTH, D_MODEL), BRANCH_WIDTH ** -0.5 * BETA)
    w_out = nrm(ks[3], (DEPTH, D_MODEL, D_MODEL), D_MODEL ** -0.5 * BETA)
    ssm_lambda_re = -0.5 + nrm(ks[4], (DEPTH, G, P), 0.01)
    ssm_lambda_im = math.pi * jnp.arange(P, dtype=f32) + nrm(ks[5], (DEPTH, G, P), 0.01)
    ssm_log_dt = jax.random.uniform(ks[6], (DEPTH, G), f32, math.log(DT_MIN), math.log(DT_MAX))
    ssm_b_re = nrm(ks[7], (DEPTH, G, P, SSM_GROUP), (2 * SSM_GROUP) ** -0.5)
    ssm_b_im = nrm(ks[8], (DEPTH, G, P, SSM_GROUP), (2 * SSM_GROUP) ** -0.5)
    ssm_c_re = nrm(ks[9], (DEPTH, G, SSM_GROUP, P), P ** -0.5)
    ssm_c_im = nrm(ks[10], (DEPTH, G, SSM_GROUP, P), P ** -0.5)
    ssm_d = nrm(ks[11], (DEPTH, SSM_WIDTH), 1.0)
    ssm_w_glu = nrm(ks[12], (DEPTH, SSM_WIDTH, 2 * SSM_WIDTH), SSM_WIDTH ** -0.5)
    diff_lambda = nrm(ks[13], (DEPTH, 4, DIFF_HEAD_DIM), 0.1)
    diff_norm_g = 1.0 + nrm(ks[14], (DEPTH, DIFF_WIDTH), 0.05)
    fox_f_bias = jax.random.uniform(ks[15], (DEPTH, FOX_HEADS), f32, 1.0, 4.0)
    ln1_g = 1.0 + nrm(ks[16], (DEPTH, D_MODEL), 0.05)
    ln1_b = nrm(ks[17], (DEPTH, D_MODEL), 0.02)
    moe_w_group = nrm(ks[18], (DEPTH, D_MODEL, N_EXPERT_GROUPS), D_MODEL ** -0.5)
    moe_b_group = nrm(ks[19], (DEPTH, N_EXPERT_GROUPS), 0.01)
    moe_w_expert = nrm(ks[20], (DEPTH, D_MODEL, N_EXPERTS), D_MODEL ** -0.5)
    moe_b_expert = nrm(ks[21], (DEPTH, N_EXPERTS), 0.01)
    moe_w_gate = nrm(ks[22], (DEPTH, N_EXPERTS, D_MODEL, D_EXPERT), D_MODEL ** -0.5)
    moe_w_up = nrm(ks[23], (DEPTH, N_EXPERTS, D_MODEL, D_EXPERT), D_MODEL ** -0.5)
    moe_w_down = nrm(ks[24], (DEPTH, N_EXPERTS, D_EXPERT, D_MODEL), D_EXPERT ** -0.5 * BETA)
    ln2_g = 1.0 + nrm(ks[25], (DEPTH, D_MODEL), 0.05)
    ln2_b = nrm(ks[26], (DEPTH, D_MODEL), 0.02)
    return {'x': x, 'w_in': w_in, 'w_branch': w_branch, 'w_out': w_out,
            'ssm_lambda_re': ssm_lambda_re, 'ssm_lambda_im': ssm_lambda_im, 'ssm_log_dt': ssm_log_dt,
            'ssm_b_re': ssm_b_re, 'ssm_b_im': ssm_b_im, 'ssm_c_re': ssm_c_re, 'ssm_c_im': ssm_c_im,
            'ssm_d': ssm_d, 'ssm_w_glu': ssm_w_glu, 'diff_lambda': diff_lambda, 'diff_norm_g': diff_norm_g,
            'fox_f_bias': fox_f_bias, 'ln1_g': ln1_g, 'ln1_b': ln1_b,
            'moe_w_group': moe_w_group, 'moe_b_group': moe_b_group,
            'moe_w_expert': moe_w_expert, 'moe_b_expert': moe_b_expert,
            'moe_w_gate': moe_w_gate, 'moe_w_up': moe_w_up, 'moe_w_down': moe_w_down,
            'ln2_g': ln2_g, 'ln2_b': ln2_b}


def reference(x, w_in, w_branch, w_out, ssm_lambda_re, ssm_lambda_im, ssm_log_dt, ssm_b_re, ssm_b_im,
              ssm_c_re, ssm_c_im, ssm_d, ssm_w_glu, diff_lambda, diff_norm_g, fox_f_bias, ln1_g, ln1_b,
              moe_w_group, moe_b_group, moe_w_expert, moe_b_expert, moe_w_gate, moe_w_up, moe_w_down,
              ln2_g, ln2_b):
    s = x.shape[1]
    pos = jnp.arange(s, dtype=jnp.float32)
    inv_freq = ROPE_THETA ** (-jnp.arange(0, DIFF_HEAD_DIM, 2, dtype=jnp.float32) / DIFF_HEAD_DIM)
    ang = pos[:, None] * inv_freq[None, :]
    emb = jnp.concatenate([ang, ang], axis=-1)
    cos = jnp.cos(emb)
    sin = jnp.sin(emb)
    for l in range(DEPTH):
        lam_init = 0.8 - 0.6 * math.exp(-0.3 * l)
        mix = token_mixer(x, w_in[l], w_branch[l], w_out[l], ssm_lambda_re[l], ssm_lambda_im[l],
                          ssm_log_dt[l], ssm_b_re[l], ssm_b_im[l], ssm_c_re[l], ssm_c_im[l], ssm_d[l],
                          ssm_w_glu[l], diff_lambda[l], diff_norm_g[l], fox_f_bias[l], lam_init, cos, sin)
        x = layer_norm(ALPHA * x + mix, ln1_g[l], ln1_b[l])
        ff = hier_moe(x, moe_w_group[l], moe_b_group[l], moe_w_expert[l], moe_b_expert[l],
                      moe_w_gate[l], moe_w_up[l], moe_w_down[l])
        x = layer_norm(ALPHA * x + ff, ln2_g[l], ln2_b[l])
    return x
```

```python
import math
import contextlib
import numpy as np
import concourse.bass as bass
import concourse.mybir as mybir
from concourse.bass_utils import run_bass_kernel_spmd

F32 = mybir.dt.float32
BF16 = mybir.dt.bfloat16
I32 = mybir.dt.int32
AF = mybir.ActivationFunctionType
ALU = mybir.AluOpType
AX = mybir.AxisListType

D = 1024
NLT = 4
D_IN = 6664
C_SSM, C_DQ, C_DK, C_DV, C_FQ, C_FK, C_FV, C_FF = 512, 1024, 1536, 2048, 2560, 3072, 3584, 3592
ALPHA = 8 ** 0.25
LN_EPS = 1e-5
RMS_EPS = 1e-6
TWO_PI = 2.0 * math.pi
ENGS = ("pe", "act", "dve", "pool", "sp")
SAME_ENG_SYNC = True
FUSE_WAIT = True


class H:
    __slots__ = ("eng", "idx", "dma_sem", "dma_val")

    def __init__(self, eng, idx, dma_sem=None, dma_val=None):
        self.eng, self.idx, self.dma_sem, self.dma_val = eng, idx, dma_sem, dma_val


class Prog:
    def __init__(self, nc, st):
        self.nc, self.st = nc, st
        self.ops = {e: [] for e in ENGS}
        self.base = {e: 0 for e in ENGS}
        self.cnt = {e: 0 for e in ENGS}
        self.esem = {e: st.enter_context(nc.semaphore("s_" + e)) for e in ENGS}
        self.dsem = {}
        NS = 32
        for k in range(NS):
            self.dsem[k] = [st.enter_context(nc.semaphore("d_%d" % k)), 0]
        self.ring_next = 0
        self.NS = NS
        self.lastw = {}
        self.readers = {}
        self.waited = {e: {} for e in ENGS}
        self.sigval = {}
        self.nphase = 0

    def _deps(self, reads, writes):
        deps = []
        for r in reads:
            w = self.lastw.get(r)
            if w is not None:
                deps.append(w)
        for r in writes:
            w = self.lastw.get(r)
            if w is not None:
                deps.append(w)
            deps.extend(self.readers.get(r, {}).values())
        return deps

    def _update(self, h, reads, writes):
        key = h.eng if h.dma_sem is None else ("dma", h.dma_sem)
        for r in reads:
            self.readers.setdefault(r, {})[key] = h
        for r in writes:
            self.lastw[r] = h
            self.readers[r] = {}

    def op(self, eng, fn, reads=(), writes=()):
        px = [r for r in reads if isinstance(r, str) and r.startswith("ps")]
        if px:
            reads = [r for r in reads if r not in px]
            writes = list(writes) + px
        deps = self._deps(reads, writes)
        lst = self.ops[eng]
        lst.append(dict(fn=fn, deps=deps, sig=False, dma=None))
        h = H(eng, self.base[eng] + len(lst) - 1)
        self._update(h, reads, writes)
        return h

    def dma(self, eng, out, in_, sem, reads=(), writes=(), **kw):
        k = self.ring_next
        self.ring_next = (k + 1) % self.NS
        prev = self.dsem[k][1]
        self.dsem[k][1] += 16
        val = self.dsem[k][1]
        deps = self._deps(reads, writes)
        if prev > 0:
            deps.append(H(eng, -1, dma_sem=k, dma_val=prev))
        lst = self.ops[eng]
        lst.append(dict(fn=lambda e: e.dma_start(out=out, in_=in_, **kw), deps=deps, sig=False, dma=k))
        h = H(eng, self.base[eng] + len(lst) - 1, dma_sem=k, dma_val=val)
        self._update(h, reads, writes)
        return h

    def flush(self, final=False):
        nc = self.nc
        for e in ENGS:
            for o in self.ops[e]:
                for d in o["deps"]:
                    if d.dma_sem is None and d.idx >= self.base[d.eng]:
                        if d.eng == "pe" and e == "pe":
                            continue
                        if d.eng == e and not SAME_ENG_SYNC:
                            continue
                        self.ops[d.eng][d.idx - self.base[d.eng]]["sig"] = True
        for e in ENGS:
            c = self.cnt[e]
            for i, o in enumerate(self.ops[e]):
                if o["sig"]:
                    c += 1
                self.sigval[(e, self.base[e] + i)] = c
            self.cnt[e] = c
        used_dma = set(o["dma"] for e in ENGS for o in self.ops[e] if o["dma"] is not None)
        self.nphase += 1
        with nc.Block(no_gpsimd_drain=(not final)) as block:
            def run(ename):
                def body(eng):
                    waited = self.waited[ename]
                    for i, o in enumerate(self.ops[ename]):
                        pend = []
                        for d in o["deps"]:
                            if d.dma_sem is not None:
                                key, val, sem = ("dma", d.dma_sem), d.dma_val, self.dsem[d.dma_sem][0]
                            else:
                                if d.idx < self.base[d.eng]:
                                    continue
                                if d.eng == "pe" and ename == "pe":
                                    continue
                                if d.eng == ename and not SAME_ENG_SYNC:
                                    continue
                                key, val, sem = d.eng, self.sigval[(d.eng, d.idx)], self.esem[d.eng]
                            if waited.get(key, 0) >= val:
                                continue
                            waited[key] = val
                            pend.append((sem, val))
                        fuse = FUSE_WAIT and o["dma"] is None and len(pend) > 0
                        for (sem, val) in (pend[:-1] if fuse else pend):
                            eng.wait_ge(sem, val)
                        ins = o["fn"](eng)
                        if fuse:
                            ins._wait_ge(pend[-1][0], pend[-1][1])
                        if o["dma"] is not None:
                            ins.then_inc(self.dsem[o["dma"]][0], 16)
                        elif o["sig"]:
                            ins.then_inc(self.esem[ename], 1)
                    if ename == "sp":
                        for name in sorted(used_dma):
                            v = self.dsem[name][1]
                            if waited.get(("dma", name), 0) < v:
                                waited[("dma", name)] = v
                                eng.wait_ge(self.dsem[name][0], v)
                return body

            block.tensor(run("pe"))
            block.scalar(run("act"))
            block.vector(run("dve"))
            block.gpsimd(run("pool"))
            block.sync(run("sp"))
        for e in ENGS:
            self.base[e] += len(self.ops[e])
            self.ops[e] = []
        self.lastw = {}
        self.readers = {}


def build(NL=4, S=2048, dbg=(), stop=None):
    NT, NB = S // 128, S // 512
    nc = bass.Bass("TRN2", target_bir_lowering=False)

    def din(name, shape):
        return nc.dram_tensor(name, list(shape), F32, kind="ExternalInput").ap()

    x_d = din("x", [S, D])
    w_in_d = din("w_in", [NLT, D, D_IN])
    w_br_d = din("w_branch", [NLT, 3 * 512, D])
    w_out_d = din("w_out", [NLT, D, D])
    lampq_d = din("lampq", [NLT, 128, 16, 3])
    lamrep_d = din("lamrep", [NLT, 128, 3, 2048])
    bblk_re_d = din("bblk_re", [NLT, 128, 16, 128])
    bblk_im_d = din("bblk_im", [NLT, 128, 16, 128])
    bblkT_re_d = din("bblkT_re", [NLT, 128, 16, 128])
    bblkT_im_d = din("bblkT_im", [NLT, 128, 16, 128])
    cblk_re_d = din("cblk_re", [NLT, 128, 16, 128])
    cblk_im_d = din("cblk_im", [NLT, 128, 16, 128])
    dpq_d = din("dpq", [NLT, 128, 4])
    wglu_d = din("w_glu", [NLT, 512, 1024])
    dlam_d = din("dlam", [NLT, 128, 4, 64])
    dg_d = din("dgrep", [NLT, 128, 512])
    fb_d = din("fb", [NLT, 8, 1])
    lnrep_d = din("lnrep", [NLT, 4, 128, D])
    wr_d = din("w_r", [NLT, D, 20])
    brep_d = din("b_rrep", [NLT, 128, 20])
    wg_d = din("moe_w_gate", [NLT, 16, D, 256])
    wu_d = din("moe_w_up", [NLT, 16, D, 256])
    wd_d = din("moe_w_down", [NLT, 16, 256, D])
    ropec_d = din("ropec", [128, S])
    ropes_d = din("ropes", [128, S])
    sel_d = din("sel", [128, 16, 128])
    out_d = nc.dram_tensor("out", [S, D], F32, kind="ExternalOutput").ap()
    dbg_d = {}
    for name, shape, dt_ in dbg:
        dbg_d[name] = nc.dram_tensor("dbg_" + name, list(shape), dt_, kind="ExternalOutput").ap()

    with contextlib.ExitStack() as st:
        uid = [0]

        def sb(name, shape, dt, stack=st):
            uid[0] += 1
            return stack.enter_context(nc.sbuf_tensor("sb%d_%s" % (uid[0], name), list(shape), dt))

        P = Prog(nc, st)
        ps = [st.enter_context(nc.psum_tensor("ps%d" % i, [128, 512], F32)) for i in range(8)]
        PSR = ["ps%d" % i for i in range(8)]

        x_scr = nc.dram_tensor("x_scr", [S, D], F32, kind="Internal").ap()
        hT = sb("hT", [128, 8, S], BF16)
        ident = sb("ident", [128, 128], F32)
        tri = sb("tri", [128, 128], BF16)
        iota_i = sb("iota_i", [128, 257], I32)
        iota_f = sb("iota_f", [128, 257], F32)

        def mm(out, lhsT, rhs, start, stop, reads, writes):
            return P.op("pe", lambda e: e.matmul(out, lhsT=lhsT, rhs=rhs, start=start, stop=stop,
                                                  skip_group_check=True), reads=reads, writes=writes)

        def act(out, in_, func, reads, writes, **kw):
            return P.op("act", lambda e: e.activation(out=out, in_=in_, func=func, **kw), reads=reads, writes=writes)

        def tt(eng, out, in0, in1, op, reads, writes):
            return P.op(eng, lambda e: e.tensor_tensor(out=out, in0=in0, in1=in1, op=op), reads=reads, writes=writes)

        def ts(eng, out, in0, s1, s2, op0, op1, reads, writes):
            if s2 is None:
                return P.op(eng, lambda e: e.tensor_scalar(out=out, in0=in0, scalar1=s1, scalar2=None, op0=op0),
                            reads=reads, writes=writes)
            return P.op(eng, lambda e: e.tensor_scalar(out=out, in0=in0, scalar1=s1, scalar2=s2, op0=op0, op1=op1),
                        reads=reads, writes=writes)

        def stt(out, in0, scalar, in1, op0, op1, reads, writes):
            return P.op("dve", lambda e: e.scalar_tensor_tensor(out=out, in0=in0, scalar=scalar, in1=in1, op0=op0, op1=op1),
                        reads=reads, writes=writes)

        def cp(eng, out, in_, reads, writes):
            if eng == "act":
                return act(out, in_, AF.Copy, reads, writes)
            return P.op(eng, lambda e: e.tensor_copy(out=out, in_=in_), reads=reads, writes=writes)

        def wload(dst, src, sem, res, eng="pool"):
            return P.dma(eng, dst, src, sem, writes=[res])

        def dump(name, src_ap, reads):
            if name in dbg_d:
                P.dma("sp", dbg_d[name], src_ap, "dbg", reads=reads)

        tr_rr = [0]

        def transposes(n, banks, src, src_res):
            for hb in range(2):
                b = banks[tr_rr[0] % len(banks)]
                tr_rr[0] += 1
                for i in range(4):
                    c = hb * 4 + i
                    P.op("pe", lambda e, b=b, i=i, c=c: e.transpose(ps[b][:, i * 128:(i + 1) * 128],
                                                                     src[:, c * 128:(c + 1) * 128], ident[:]),
                         reads=[src_res, "ident"], writes=[PSR[b]])
                cp("act", hT[:, hb * 4:(hb + 1) * 4, n * 128:(n + 1) * 128],
                   ps[b][:].rearrange("p (c t) -> p c t", c=4), reads=[PSR[b]], writes=[("hT", n // 4)])

        def interleave(gens):
            gens = list(gens)
            while gens:
                for g in list(gens):
                    try:
                        next(g)
                    except StopIteration:
                        gens.remove(g)

        def layer_norm(dst_ap, dst_res, z_ap, z_res, grep, brep, stp, k, extra=(), inplace=False):
            stt_ = stp["st"][k]
            mv = stp["mv"][k]
            xn = None if inplace else stp["xn"][k]
            R = lambda nm: (nm, k)
            for i in range(2):
                P.op("dve", lambda e, i=i: e.bn_stats(out=stt_[:, i, :], in_=z_ap[:, i * 512:(i + 1) * 512]),
                     reads=[z_res], writes=[R("lnst")])
                yield
            P.op("dve", lambda e: e.bn_aggr(out=mv[:, 0:2], in_=stt_[:].rearrange("p a b -> p (a b)")),
                 reads=[R("lnst")], writes=[R("lnmv")])
            yield
            act(mv[:, 2:3], mv[:, 1:2], AF.Ln, [R("lnmv")], [R("lnmv2")], bias=stp["eps"][:, 0:1])
            yield
            act(mv[:, 3:4], mv[:, 2:3], AF.Exp, [R("lnmv2")], [R("lnmv3")], scale=-0.5)
            yield
            stt(mv[:, 4:5], mv[:, 0:1], -1.0, mv[:, 3:4], ALU.mult, ALU.mult, [R("lnmv"), R("lnmv3")], [R("lnmv4")])
            yield
            if inplace:
                act(dst_ap, z_ap, AF.Identity, [z_res, R("lnmv3"), R("lnmv4")], [dst_res], scale=mv[:, 3:4], bias=mv[:, 4:5])
                yield
                tt("dve", dst_ap, dst_ap, grep, ALU.mult, [dst_res, "lng"], [dst_res])
                yield
                tt("dve", dst_ap, dst_ap, brep, ALU.add, [dst_res, "lnb"], [dst_res])
                yield
                return
            act(xn[:], z_ap, AF.Identity, [z_res, R("lnmv3"), R("lnmv4")], [R("lnxn")] + list(extra), scale=mv[:, 3:4], bias=mv[:, 4:5])
            yield
            tt("dve", xn[:], xn[:], grep, ALU.mult, [R("lnxn"), "lng"], [R("lnxn")])
            yield
            tt("dve", dst_ap, xn[:], brep, ALU.add, [R("lnxn"), "lnb", z_res], [dst_res])
            yield

        P.op("pool", lambda e: e.memset(ident[:], 1.0), writes=["ident"])
        P.op("pool", lambda e: e.affine_select(out=ident[:], in_=ident[:], pattern=[[-1, 128]], base=0,
                                               channel_multiplier=1, compare_op=ALU.is_equal, fill=0.0),
             reads=["ident"], writes=["ident"])
        P.op("pool", lambda e: e.memset(tri[:], 1.0), writes=["tri"])
        P.op("pool", lambda e: e.affine_select(out=tri[:], in_=tri[:], pattern=[[1, 128]], base=0,
                                               channel_multiplier=-1, compare_op=ALU.is_ge, fill=0.0),
             reads=["tri"], writes=["tri"])
        P.op("pool", lambda e: e.iota(iota_i[:], pattern=[[1, 257]], base=0, channel_multiplier=0), writes=["iota_i"])
        cp("dve", iota_f[:], iota_i[:], ["iota_i"], ["iota_f"])
        with contextlib.ExitStack() as ph:
            xin = [sb("xin%d" % i, [128, D], F32, ph) for i in range(3)]
            for n in range(NT):
                P.dma("sp", xin[n % 3][:], x_d[n * 128:(n + 1) * 128, :], "ldx%d" % (n % 3), writes=[("xin", n % 3)])
                transposes(n, [0, 1, 2, 3], xin[n % 3], ("xin", n % 3))
            P.flush()

        for l in range(NL):
            lam_init = 0.8 - 0.6 * math.exp(-0.3 * l)
            w_in_v = w_in_d[l].rearrange("(k p) c -> p k c", p=128)

            lay = contextlib.ExitStack()
            yT0 = sb("yT0", [128, 4, S], BF16, lay)
            with contextlib.ExitStack() as ph:
                NC_ = 128
                lpq = sb("lpq", [128, 16, 3], F32, ph)
                pq = sb("pq", [128, 24, 16], F32, ph)
                tabC = sb("tabC", [128, 16, NC_ + 1], F32, ph)
                tabS = sb("tabS", [128, 16, NC_ + 1], F32, ph)
                CKr = [sb("CKr%d" % i, [128, 16, 128], BF16, ph) for i in range(4)]
                CKi = [sb("CKi%d" % i, [128, 16, 128], BF16, ph) for i in range(4)]
                KT = [sb("KT%d" % i, [128, 4, 128], BF16, ph) for i in range(4)]
                BBr = [sb("BBr%d" % i, [128, 16, 128], BF16, ph) for i in range(4)]
                BBi = [sb("BBi%d" % i, [128, 16, 128], BF16, ph) for i in range(4)]
                dpq = sb("dpq", [128, 4], F32, ph)
                carry = sb("carry", [128, 16, 4], F32, ph)
                Xc = sb("Xc", [128, 2, 16], BF16, ph)
                halfpi = sb("halfpi", [128, 1], F32, ph)
                P.dma("sp", lpq[:], lampq_d[l], "ldp", writes=["lpq"])
                P.dma("sp", dpq[:], dpq_d[l], "ldp", writes=["dpq"])
                P.op("dve", lambda e: e.memset(halfpi[:], math.pi / 2), writes=["halfpi"])
                P.op("dve", lambda e: e.memset(Xc[:], 0.0), writes=["Xc"])
                PQ = lambda i: pq[:, i, :]
                act(PQ(0), lpq[:, :, 2], AF.Exp, ["lpq"], ["pq"])
                tt("dve", PQ(1), lpq[:, :, 0], PQ(0), ALU.mult, ["lpq", "pq"], ["pq"])
                act(PQ(1), PQ(1), AF.Exp, ["pq"], ["pq"])
                tt("dve", PQ(2), lpq[:, :, 1], PQ(0), ALU.mult, ["lpq", "pq"], ["pq"])
                with contextlib.ExitStack() as ph2:
                    NTMP = 516
                    tA = sb("tA", [128, NTMP], F32, ph2)
                    tB = sb("tB", [128, NTMP], F32, ph2)
                    tI = sb("tI", [128, NTMP], I32, ph2)
                    angt = sb("angt", [128, NTMP], F32, ph2)

                    TT_ = dict(tA=tA, tB=tB, tI=tI)

                    def sincos(ang_ap, n, out_c, out_s, res_in, res_c, res_s):
                        a_, b_, i_ = TT_["tA"][:, 0:n], TT_["tB"][:, 0:n], TT_["tI"][:, 0:n]
                        ts("dve", i_, ang_ap, 1.0 / TWO_PI, None, ALU.mult, None, [res_in], ["tI"])
                        cp("dve", a_, i_, ["tI"], ["tA"])
                        stt(a_, a_, -TWO_PI, ang_ap, ALU.mult, ALU.add, ["tA", res_in], ["tA"])
                        act(b_, a_, AF.Sin, ["tA"], ["tB"], scale=0.25, bias=halfpi[:, 0:1])
                        act(a_, a_, AF.Sin, ["tA", "tB"], ["tA"], scale=0.25)
                        tt("dve", b_, b_, a_, ALU.mult, ["tA", "tB"], ["tB"])
                        tt("dve", a_, a_, a_, ALU.mult, ["tA", "tB"], ["tA"])
                        ts("dve", a_, a_, -2.0, 1.0, ALU.mult, ALU.add, ["tA"], ["tA"])
                        stt(out_s, b_, 4.0, a_, ALU.mult, ALU.mult, ["tA", "tB"], [res_s])
                        tt("dve", b_, b_, b_, ALU.mult, ["tB", res_s], ["tB"])
                        ts("dve", out_c, b_, -8.0, 1.0, ALU.mult, ALU.add, ["tB"], [res_c])

                    def cmul(o_r, o_i, a_r, a_i, b_r, b_i, t1_, t2_, res):
                        tt("dve", t1_, a_r, b_r, ALU.mult, res, res)
                        tt("dve", t2_, a_i, b_i, ALU.mult, res, res)
                        tt("dve", o_r, t1_, t2_, ALU.subtract, res, res)
                        tt("dve", t1_, a_r, b_i, ALU.mult, res, res)
                        tt("dve", t2_, a_i, b_r, ALU.mult, res, res)
                        tt("dve", o_i, t1_, t2_, ALU.add, res, res)

                    sincos(PQ(2), 16, PQ(3), PQ(4), "pq", "pq", "pq")
                    tt("dve", PQ(5), PQ(3), PQ(1), ALU.mult, ["pq"], ["pq"])
                    tt("dve", PQ(6), PQ(4), PQ(1), ALU.mult, ["pq"], ["pq"])
                    cmul(PQ(7), PQ(8), PQ(5), PQ(6), PQ(5), PQ(6), PQ(15), PQ(16), ["pq"])
                    cmul(PQ(9), PQ(10), PQ(7), PQ(8), PQ(5), PQ(6), PQ(15), PQ(16), ["pq"])
                    cmul(PQ(11), PQ(12), PQ(9), PQ(10), PQ(5), PQ(6), PQ(15), PQ(16), ["pq"])
                    tt("dve", PQ(20), PQ(1), PQ(1), ALU.mult, ["pq"], ["pq"])
                    tt("dve", PQ(20), PQ(20), PQ(20), ALU.mult, ["pq"], ["pq"])
                    ts("dve", PQ(21), PQ(2), 4.0, None, ALU.mult, None, ["pq"], ["pq"])
                    for hf in range(4):
                        js = slice(hf * 4, hf * 4 + 4)
                        tt("dve", angt[:].rearrange("p (j s) -> p j s", j=4),
                           iota_f[:, 0:NC_ + 1].unsqueeze(1).to_broadcast([128, 4, NC_ + 1]),
                           pq[:, 21, js].unsqueeze(2).to_broadcast([128, 4, NC_ + 1]), ALU.mult, ["iota_f", "pq"], ["angt"])
                        sincos(angt[:], 4 * (NC_ + 1), tabC[:, js, :].rearrange("p j s -> p (j s)"),
                               tabS[:, js, :].rearrange("p j s -> p (j s)"), "angt", "tabC", "tabS")
                    lrq, liq = lpq[:, :, 0], lpq[:, :, 1]
                    ts("dve", PQ(17), PQ(5), -1.0, None, ALU.add, None, ["pq"], ["pq"])
                    tt("dve", PQ(15), lrq, lrq, ALU.mult, ["lpq", "pq"], ["pq"])
                    tt("dve", PQ(16), liq, liq, ALU.mult, ["lpq", "pq"], ["pq"])
                    tt("dve", PQ(15), PQ(15), PQ(16), ALU.add, ["pq"], ["pq"])
                    P.op("dve", lambda e: e.reciprocal(out=PQ(18), in_=PQ(15)), reads=["pq"], writes=["pq"])
                    tt("dve", PQ(15), PQ(17), lrq, ALU.mult, ["pq", "lpq"], ["pq"])
                    tt("dve", PQ(16), PQ(6), liq, ALU.mult, ["pq", "lpq"], ["pq"])
                    tt("dve", PQ(15), PQ(15), PQ(16), ALU.add, ["pq"], ["pq"])
                    tt("dve", PQ(13), PQ(15), PQ(18), ALU.mult, ["pq"], ["pq"])
                    tt("dve", PQ(15), PQ(6), lrq, ALU.mult, ["pq", "lpq"], ["pq"])
                    tt("dve", PQ(16), PQ(17), liq, ALU.mult, ["pq", "lpq"], ["pq"])
                    tt("dve", PQ(15), PQ(15), PQ(16), ALU.subtract, ["pq"], ["pq"])
                    tt("dve", PQ(14), PQ(15), PQ(18), ALU.mult, ["pq"], ["pq"])
                    apow = [(PQ(5), PQ(6)), (PQ(7), PQ(8)), (PQ(9), PQ(10)), (PQ(11), PQ(12))]
                    with contextlib.ExitStack() as ph3:
                        SA = []
                        for k in range(2):
                            SA.append(dict(cre=sb("cre%d" % k, [128, 4, 128], F32, ph3), cim=sb("cim%d" % k, [128, 4, 128], F32, ph3),
                                           k1=sb("k1_%d" % k, [128, 4, 128], F32, ph3), k2=sb("k2_%d" % k, [128, 4, 128], F32, ph3),
                                           bTr=sb("bTr%d" % k, [128, 4, 128], F32, ph3), bTi=sb("bTi%d" % k, [128, 4, 128], F32, ph3),
                                           kr=sb("kr%d" % k, [128, 4, 128], F32, ph3), ki=sb("ki%d" % k, [128, 4, 128], F32, ph3)))
                        C0r = sb("C0r", [128, 16, 128], BF16, ph3)
                        C0i = sb("C0i", [128, 16, 128], BF16, ph3)
                        BTrs = [sb("BTr%d" % i, [128, 16, 128], BF16, ph3) for i in range(2)]
                        BTis = [sb("BTi%d" % i, [128, 16, 128], BF16, ph3) for i in range(2)]
                        bc = lambda ap, js: ap[:, js].unsqueeze(2).to_broadcast([128, 4, 128])

                        def stageA(hf, k):
                            T = SA[k]
                            N = lambda nm: (nm, k)
                            cre, cim, k1, k2 = T["cre"], T["cim"], T["k1"], T["k2"]
                            js = slice(hf * 4, hf * 4 + 4)
                            P.dma("sp", cre[:], cblk_re_d[l][:, js, :], "ld", writes=[N("cre")])
                            P.dma("sp", cim[:], cblk_im_d[l][:, js, :], "ld", writes=[N("cim")])
                            cp("act", C0r[:, js, :], cre[:], [N("cre")], ["C0r"]); yield
                            act(C0i[:, js, :], cim[:], AF.Copy, [N("cim")], ["C0i"], scale=-1.0); yield
                            for jo in range(4):
                                pr, pi = apow[jo]
                                tt("dve", k1[:], cre[:], bc(pr, js), ALU.mult, [N("cre"), "pq"], [N("k1")]); yield
                                tt("dve", k2[:], cim[:], bc(pi, js), ALU.mult, [N("cim"), "pq"], [N("k2")]); yield
                                tt("dve", CKr[jo][:, js, :], k1[:], k2[:], ALU.subtract, [N("k1"), N("k2")], [("CK", jo)]); yield
                                tt("dve", k1[:], cre[:], bc(pi, js), ALU.mult, [N("cre"), "pq", N("k1")], [N("k1")]); yield
                                tt("dve", k2[:], cim[:], bc(pr, js), ALU.mult, [N("cim"), "pq", N("k2")], [N("k2")]); yield
                                stt(CKi[jo][:, js, :], k1[:], -1.0, k2[:], ALU.mult, ALU.subtract, [N("k1"), N("k2")], [("CK", jo)]); yield

                        interleave([stageA(0, 0), stageA(1, 1)])
                        interleave([stageA(2, 0), stageA(3, 1)])

                        def stageB(tau, hf, k, qr_, qi_):
                            T = SA[k]
                            N = lambda nm: (nm, k)
                            bTr, bTi, k1, k2, kr, ki = T["bTr"], T["bTi"], T["k1"], T["k2"], T["kr"], T["ki"]
                            BTr, BTi = BTrs[tau % 2], BTis[tau % 2]
                            js = slice(hf * 4, hf * 4 + 4)
                            P.dma("sp", bTr[:], bblkT_re_d[l][:, js, :], "ld", writes=[N("bTr")])
                            P.dma("sp", bTi[:], bblkT_im_d[l][:, js, :], "ld", writes=[N("bTi")])
                            tt("dve", k1[:], bTr[:], bc(qr_, js), ALU.mult, [N("bTr"), "pq", N("k1")], [N("k1")]); yield
                            tt("dve", k2[:], bTi[:], bc(qi_, js), ALU.mult, [N("bTi"), "pq", N("k2")], [N("k2")]); yield
                            tt("dve", kr[:], k1[:], k2[:], ALU.subtract, [N("k1"), N("k2")], [N("kr")]); yield
                            cp("act", BTr[:, js, :], kr[:], [N("kr")], [("BTr", tau % 2)])
                            br_ = 2 + 2 * k
                            for jq in range(4):
                                P.op("pe", lambda e, jq=jq: e.transpose(ps[br_][:, jq * 128:(jq + 1) * 128], kr[:, jq, :], ident[:]),
                                     reads=[N("kr"), "ident"], writes=[PSR[br_]])
                            cp("act", BBr[tau][:, js, :].rearrange("p j c -> p (j c)"), ps[br_][:], [PSR[br_]], [("BB", tau)]); yield
                            tt("dve", k1[:], bTi[:], bc(qr_, js), ALU.mult, [N("bTi"), "pq", N("k1")], [N("k1")]); yield
                            tt("dve", k2[:], bTr[:], bc(qi_, js), ALU.mult, [N("bTr"), "pq", N("k2")], [N("k2")]); yield
                            tt("dve", ki[:], k1[:], k2[:], ALU.add, [N("k1"), N("k2")], [N("ki")]); yield
                            cp("act", BTi[:, js, :], ki[:], [N("ki")], [("BTi", tau % 2)])
                            bi_ = 3 + 2 * k
                            for jq in range(4):
                                P.op("pe", lambda e, jq=jq: e.transpose(ps[bi_][:, jq * 128:(jq + 1) * 128], ki[:, jq, :], ident[:]),
                                     reads=[N("ki"), "ident"], writes=[PSR[bi_]])
                            cp("act", BBi[tau][:, js, :].rearrange("p j c -> p (j c)"), ps[bi_][:], [PSR[bi_]], [("BB", tau)]); yield

                        for tau in range(4):
                            if tau == 0:
                                qr_, qi_ = PQ(13), PQ(14)
                            else:
                                cmul(pq[:, 22 + 0, :] if False else PQ(22), PQ(23), PQ(13), PQ(14), apow[tau - 1][0], apow[tau - 1][1], PQ(15), PQ(16), ["pq"])
                                qr_, qi_ = PQ(22), PQ(23)
                            interleave([stageB(tau, 0, 0, qr_, qi_), stageB(tau, 1, 1, qr_, qi_)])
                            interleave([stageB(tau, 2, 0, qr_, qi_), stageB(tau, 3, 1, qr_, qi_)])
                            BTr, BTi = BTrs[tau % 2], BTis[tau % 2]
                            for c4 in range(4):
                                for q4 in range(4):
                                    jg = c4 * 4 + q4
                                    mm(ps[tau % 2][:, c4 * 128:(c4 + 1) * 128], BTr[:, jg, :], C0r[:, jg, :], q4 == 0, False,
                                       [("BTr", tau % 2), "C0r"], [PSR[tau % 2]])
                                    mm(ps[tau % 2][:, c4 * 128:(c4 + 1) * 128], BTi[:, jg, :], C0i[:, jg, :], False, q4 == 3,
                                       [("BTi", tau % 2), "C0i"], [PSR[tau % 2]])
                            cp("act", KT[tau][:].rearrange("p c n -> p (c n)"), ps[tau % 2][:], [PSR[tau % 2]], [("KT", tau)])
                        P.flush()
                with contextlib.ExitStack() as ph2:
                    w_u = sb("w_u", [128, 8, 512], BF16, ph2)
                    wglu = sb("wglu", [128, 4, 1024], BF16, ph2)
                    wload(w_u[:], w_in_v[:, :, 0:512], "w0", "w_u")
                    wload(wglu[:], wglu_d[l].rearrange("(c p) d -> p c d", p=128), "w1", "wglu")
                    u_bfs = [sb("u_bf%d" % i, [128, 4, 512], BF16, ph2) for i in range(2)]
                    dus = [sb("du%d" % i, [128, 4, 512], F32, ph2) for i in range(2)]
                    XR = sb("XR", [128, 3, 4, NC_ + 2], BF16, ph2)
                    XI = sb("XI", [128, 3, 4, NC_ + 2], BF16, ph2)
                    YG = sb("YG", [128, 4, 512], BF16, ph2)
                    mk = lambda nm: [sb("%s_%d" % (nm, i), [128, NC_], F32, ph2) for i in range(4)]
                    t1, t2, wr_, wi_, vr_, vi_, p1, p2 = mk("t1"), mk("t2"), mk("wr"), mk("wi"), mk("vr"), mk("vi"), mk("p1"), mk("p2")
                    yy = sb("yy", [128, 512], F32, ph2)
                    y2 = sb("y2", [128, 512], F32, ph2)
                    ssm_pend = []
                    for tb in range(NB):
                        tcols = slice(tb * 512, (tb + 1) * 512)
                        u_bf, du, ub = u_bfs[tb % 2], dus[tb % 2], tb % 2
                        for c4 in range(4):
                            b = c4 % 2
                            for k in range(8):
                                mm(ps[b][:], w_u[:, k, c4 * 128:(c4 + 1) * 128], hT[:, k, tcols], k == 0, k == 7,
                                   ["w_u", ("hT", tb)], [PSR[b]])
                            act(u_bf[:, c4, :], ps[b][:], AF.Copy, [PSR[b]], [("u_bf", ub, c4)])
                            act(du[:, c4, :], ps[b][:], AF.Copy, [PSR[b], "dpq"], [("du", ub, c4)], scale=dpq[:, c4:c4 + 1])
                        u4 = u_bf[:].rearrange("p c (n f) -> p c n f", f=4)
                        def ssm_j(j, tb=tb, u4=u4, tcols=tcols, ub=ub):
                            c4, jj, par = j // 4, j % 4, (tb * 4 + j // 4) % 3
                            b4 = 2 + (j % 4)
                            for i in range(4):
                                mm(ps[b4][:, 0:NC_], BBr[3 - i][:, j, :], u4[:, c4, :, i], i == 0, i == 3,
                                   [("BB", 3 - i), ("u_bf", ub, c4)], [PSR[b4]])
                            for i in range(4):
                                mm(ps[b4][:, NC_:2 * NC_], BBi[3 - i][:, j, :], u4[:, c4, :, i], i == 0, i == 3,
                                   [("BB", 3 - i), ("u_bf", ub, c4)], [PSR[b4]])
                            s_ = j % 4
                            cT, sT = tabC[:, j, 0:NC_], tabS[:, j, 0:NC_]
                            R = lambda nm: (nm, s_)
                            tt("dve", t1[s_][:], cT, ps[b4][:, 0:NC_], ALU.mult, ["tabC", PSR[b4]], [R("t1")])
                            yield
                            tt("dve", t2[s_][:], sT, ps[b4][:, NC_:2 * NC_], ALU.mult, ["tabS", PSR[b4]], [R("t2")])
                            yield
                            tt("dve", wr_[s_][:], t1[s_][:], t2[s_][:], ALU.add, [R("t1"), R("t2")], [R("wr")])
                            yield
                            tt("dve", t1[s_][:], cT, ps[b4][:, NC_:2 * NC_], ALU.mult, ["tabC", PSR[b4], R("t1")], [R("t1")])
                            yield
                            tt("dve", t2[s_][:], sT, ps[b4][:, 0:NC_], ALU.mult, ["tabS", PSR[b4], R("t2")], [R("t2")])
                            yield
                            tt("dve", wi_[s_][:], t1[s_][:], t2[s_][:], ALU.subtract, [R("t1"), R("t2")], [R("wi")])
                            yield
                            if tb > 0:
                                cE, sE = tabC[:, j, NC_:NC_ + 1], tabS[:, j, NC_:NC_ + 1]
                                ts("dve", carry[:, j, 2:3], carry[:, j, 1:2], sE, None, ALU.mult, None,
                                   [("carry", j), "tabS"], [("cinit", j)])
                                stt(carry[:, j, 2:3], carry[:, j, 0:1], cE, carry[:, j, 2:3], ALU.mult, ALU.subtract,
                                    [("carry", j), ("cinit", j), "tabC"], [("cinit", j)])
                                ts("dve", carry[:, j, 3:4], carry[:, j, 0:1], sE, None, ALU.mult, None,
                                   [("carry", j), "tabS"], [("cinit2", j)])
                                stt(carry[:, j, 3:4], carry[:, j, 1:2], cE, carry[:, j, 3:4], ALU.mult, ALU.add,
                                    [("carry", j), ("cinit2", j), "tabC"], [("cinit2", j)])
                                ini_r, ini_i = carry[:, j, 2:3], carry[:, j, 3:4]
                            else:
                                ini_r, ini_i = 0.0, 0.0
                            magb = pq[:, 20, j:j + 1].to_broadcast([128, NC_])
                            P.op("dve", lambda e, s_=s_, magb=magb, ini_r=ini_r: e.tensor_tensor_scan(
                                out=vr_[s_][:], data0=magb, data1=wr_[s_][:], initial=ini_r, op0=ALU.mult, op1=ALU.add),
                                reads=[R("wr"), "pq", ("cinit", j)], writes=[R("vr")])
                            yield
                            P.op("dve", lambda e, s_=s_, magb=magb, ini_i=ini_i: e.tensor_tensor_scan(
                                out=vi_[s_][:], data0=magb, data1=wi_[s_][:], initial=ini_i, op0=ALU.mult, op1=ALU.add),
                                reads=[R("wi"), "pq", ("cinit2", j)], writes=[R("vi")])
                            yield
                            cp("dve", carry[:, j, 0:1], vr_[s_][:, NC_ - 1:NC_], [R("vr")], [("carry", j)])
                            yield
                            cp("dve", carry[:, j, 1:2], vi_[s_][:, NC_ - 1:NC_], [R("vi"), ("carry", j)], [("carry", j)])
                            yield
                            xres = ("X", par, jj)
                            cp("pool", XR[:, par, jj, 0:1], Xc[:, 0, j:j + 1], [("Xc", j)], [xres])
                            yield
                            cp("pool", XI[:, par, jj, 0:1], Xc[:, 1, j:j + 1], [("Xc", j), xres], [xres])
                            yield
                            tt("pool", p1[s_][:], cT, vr_[s_][:], ALU.mult, ["tabC", R("vr")], [R("p1")])
                            yield
                            tt("pool", p2[s_][:], sT, vi_[s_][:], ALU.mult, ["tabS", R("vi")], [R("p2")])
                            yield
                            tt("pool", XR[:, par, jj, 1:NC_ + 1], p1[s_][:], p2[s_][:], ALU.subtract, [R("p1"), R("p2"), xres], [xres])
                            yield
                            tt("pool", p1[s_][:], cT, vi_[s_][:], ALU.mult, ["tabC", R("vi"), R("p1")], [R("p1")])
                            yield
                            tt("pool", p2[s_][:], sT, vr_[s_][:], ALU.mult, ["tabS", R("vr"), R("p2")], [R("p2")])
                            yield
                            tt("pool", XI[:, par, jj, 1:NC_ + 1], p1[s_][:], p2[s_][:], ALU.add, [R("p1"), R("p2"), xres], [xres])
                            yield
                            cp("pool", Xc[:, 0, j:j + 1], XR[:, par, jj, NC_:NC_ + 1], [xres], [("Xc", j)])
                            yield
                            cp("pool", Xc[:, 1, j:j + 1], XI[:, par, jj, NC_:NC_ + 1], [xres, ("Xc", j)], [("Xc", j)])
                            yield
                            yield
                        def ssm_back(c4, tb=tb, u4=u4, tcols=tcols, ub=ub, du=du):
                            par = (tb * 4 + c4) % 3
                            yb = 6 + (c4 % 2)
                            for jo in range(4):
                                reg = ps[yb][:, jo * NC_:(jo + 1) * NC_]
                                for q4 in range(4):
                                    jg = c4 * 4 + q4
                                    mm(reg, CKr[jo][:, jg, :], XR[:, par, q4, 0:NC_], q4 == 0, False, [("CK", jo), ("X", par, q4)], [PSR[yb]])
                                    mm(reg, CKi[jo][:, jg, :], XI[:, par, q4, 0:NC_], False, False, [("CK", jo), ("X", par, q4)], [PSR[yb]])
                                for i in range(jo + 1):
                                    mm(reg, KT[jo - i][:, c4, :], u4[:, c4, :, i], False, i == jo, [("KT", jo - i), ("u_bf", ub, c4)], [PSR[yb]])
                                yield
                            tt("dve", yy[:].rearrange("p (c f) -> p c f", f=4), ps[yb][:].rearrange("p (f c) -> p c f", f=4),
                               du[:, c4, :].rearrange("p (c f) -> p c f", f=4), ALU.add, [PSR[yb], ("du", ub, c4)], ["yy"])
                            yield
                            tt("dve", y2[:], yy[:], yy[:], ALU.mult, ["yy"], ["y2"])
                            yield
                            ts("dve", y2[:], y2[:], 0.044715, 1.0, ALU.mult, ALU.add, ["y2"], ["y2"])
                            yield
                            tt("dve", y2[:], y2[:], yy[:], ALU.mult, ["y2", "yy"], ["y2"])
                            yield
                            act(y2[:], y2[:], AF.Sigmoid, ["y2"], ["y2"], scale=1.5957691216057308)
                            yield
                            tt("dve", YG[:, c4, :], y2[:], yy[:], ALU.mult, ["y2", "yy"], [("YG", c4)])
                            yield

                            if c4 == 3:
                                for wc in range(4):
                                    bv, bg = (0, 1)
                                    for cg in range(4):
                                        mm(ps[bv][:], wglu[:, cg, wc * 128:(wc + 1) * 128], YG[:, cg, :], cg == 0, cg == 3,
                                           ["wglu", ("YG", cg)], [PSR[bv]])
                                    for cg in range(4):
                                        mm(ps[bg][:], wglu[:, cg, 512 + wc * 128:512 + (wc + 1) * 128], YG[:, cg, :], cg == 0, cg == 3,
                                           ["wglu", ("YG", cg)], [PSR[bg]])
                                    act(y2[:], ps[bg][:], AF.Sigmoid, [PSR[bg]], ["y2"])
                                    yield
                                    tt("dve", yT0[:, wc, tcols], ps[bv][:], y2[:], ALU.mult, [PSR[bv], "y2"], [("yT", wc, tb)])
                                    yield
                        for c4 in range(4):
                            gens = [ssm_j(c4 * 4 + i) for i in range(4)]
                            if len(ssm_pend) >= 2:
                                gens.append(ssm_pend.pop(0))
                            interleave(gens)
                            ssm_pend.append(ssm_back(c4))
                    for g_ in ssm_pend:
                        interleave([g_])
                    P.flush()
            if stop == "ssm":
                P.dma("sp", dbg_d["yT"][:, 0:4, :], yT0[:], "dbg", reads=[])
                P.flush()
                lay.close()
                break
            def attention(qT_ap_fn, kT_ap_fn, V_ap_fn, dvp, bias_fn, scale, sbanks, accbank_fn, pts, qb, rres, tagw):
                nk = 4 * qb + 4
                LAG = 2
                started = set()
                slots = {}
                for step in range(nk + LAG):
                    if step < nk:
                        kt = step
                        r = kt - 4 * qb
                        c0 = max(0, r) * 128
                        sl_ = attention.rr % 3
                        attention.rr += 1
                        sbk = sbanks[sl_]
                        pt = pts[sl_]
                        ptres = ("pt", sl_)
                        slots[kt] = (pt, ptres)
                        mm(ps[sbk][:, c0:512], kT_ap_fn(kt), qT_ap_fn(slice(qb * 512 + c0, qb * 512 + 512)), True, True,
                           rres, [PSR[sbk]])
                        kw = dict(scale=scale)
                        if bias_fn is not None:
                            kw["bias"] = bias_fn(kt)
                        act(pt[:, c0:512], ps[sbk][:, c0:512], AF.Exp, [PSR[sbk]] + rres, [ptres], **kw)
                        if r >= 0:
                            tt("dve", pt[:, c0:c0 + 128], pt[:, c0:c0 + 128], tri[:], ALU.mult, [ptres, "tri"], [ptres])
                    kt = step - LAG
                    if kt >= 0:
                        r = kt - 4 * qb
                        pt, ptres = slots[kt]
                        for qs in range(max(0, r), 4):
                            bk, ap = accbank_fn(qs)
                            first = bk not in started
                            started.add(bk)
                            mm(ap, pt[:, qs * 128:(qs + 1) * 128], V_ap_fn(kt), first, kt == 4 * qb + qs,
                               [ptres] + rres, [PSR[bk]])
                    yield

            pend = [None]

            def run_unit(att_gen, epi_gen):
                gens = [att_gen] + ([pend[0]] if pend[0] is not None else [])
                interleave(gens)
                pend[0] = epi_gen

            def drain_pending():
                if pend[0] is not None:
                    interleave([pend[0]])
                    pend[0] = None

            def rr2(gs):
                gs = list(gs)
                while gs:
                    for g in list(gs):
                        try:
                            next(g)
                        except StopIteration:
                            gs.remove(g)
                        yield
            attention.rr = 0

            yT1 = sb("yT1", [128, 4, S], BF16, lay)
            with contextlib.ExitStack() as ph:
                wq = sb("wq", [128, 8, 512], BF16, ph)
                wk = sb("wk", [128, 8, 512], BF16, ph)
                wv = sb("wv", [128, 8, 512], BF16, ph)
                wqr = sb("wqr", [128, 8, 512], BF16, ph)
                wkr = sb("wkr", [128, 8, 512], BF16, ph)
                ropec = sb("ropec", [128, S], F32, ph)
                ropes = sb("ropes", [128, S], F32, ph)
                qT = sb("qT", [128, S], BF16, ph)
                kT = sb("kT", [128, S], BF16, ph)
                kTz = [sb("kTz%d" % i, [128, S], BF16, ph) for i in range(2)]
                P.op("pool", lambda e: e.memset(kTz[0][64:128, :], 0.0), writes=["kTz0"])
                P.op("pool", lambda e: e.memset(kTz[1][0:64, :], 0.0), writes=["kTz1"])
                Vp = sb("Vp", [128, NT, 4, 129], BF16, ph)
                pts = [sb("pt%d" % i, [128, 512], BF16, ph) for i in range(3)]
                ldr = sb("ldr", [128, 4, 64], F32, ph)
                lsc = sb("lsc", [128, 8], F32, ph)
                gsc = sb("gsc", [128, 512], F32, ph)
                rt1 = sb("rt1", [128, 512], F32, ph)
                rt2 = sb("rt2", [128, 512], F32, ph)
                rt3 = sb("rt3", [128, 512], F32, ph)
                rt4 = sb("rt4", [128, 512], F32, ph)
                o1 = sb("o1", [128, 4, 128], F32, ph)
                ods = [sb("od%d" % i, [128, 128], F32, ph) for i in range(2)]
                onfs = [sb("onf%d" % i, [128, 128], F32, ph) for i in range(2)]
                junks = [sb("junk%d" % i, [128, 128], F32, ph) for i in range(2)]
                sms = [sb("sm%d" % i, [128, 8], F32, ph) for i in range(2)]
                epsr = sb("epsr", [128, 1], F32, ph)
                P.op("dve", lambda e: e.memset(epsr[:], RMS_EPS), writes=["epsr"])
                P.dma("sp", ropec[:], ropec_d, "ldc", writes=["ropec"])
                P.dma("sp", ropes[:], ropes_d, "ldc", writes=["ropes"])
                P.dma("sp", ldr[:], dlam_d[l], "ldp", writes=["ldr"])
                P.dma("sp", gsc[:], dg_d[l], "ldp", writes=["gsc"])
                wload(wq[:], w_in_v[:, :, C_SSM:C_SSM + 512], "w0", "wq")
                wload(wk[:], w_in_v[:, :, C_DQ:C_DQ + 512], "w1", "wk")
                wload(wv[:], w_in_v[:, :, C_DK:C_DK + 512], "w2", "wv")
                ts("dve", gsc[:], gsc[:], 1.0 - lam_init, None, ALU.mult, None, ["gsc"], ["gsc"])
                tt("dve", ldr[:, 0, :], ldr[:, 0, :], ldr[:, 1, :], ALU.mult, ["ldr"], ["ldr"])
                tt("dve", ldr[:, 2, :], ldr[:, 2, :], ldr[:, 3, :], ALU.mult, ["ldr"], ["ldr"])
                P.op("dve", lambda e: e.reduce_sum(out=lsc[:, 0:1], in_=ldr[:, 0, :], axis=AX.X), reads=["ldr"], writes=["lsc"])
                P.op("dve", lambda e: e.reduce_sum(out=lsc[:, 1:2], in_=ldr[:, 2, :], axis=AX.X), reads=["ldr", "lsc"], writes=["lsc"])
                act(lsc[:, 2:4], lsc[:, 0:2], AF.Exp, ["lsc"], ["lsc"])
                tt("dve", lsc[:, 4:5], lsc[:, 3:4], lsc[:, 2:3], ALU.subtract, ["lsc"], ["lsc"])
                ts("dve", lsc[:, 5:6], lsc[:, 4:5], -lam_init, None, ALU.add, None, ["lsc"], ["lsc"])
                neglam = lsc[:, 5:6]
                for (w_, wr__, nm) in ((wq, wqr, "wq"), (wk, wkr, "wk")):
                    v_in = w_[:].rearrange("p k (m two f) -> p (k m) two f", two=2, f=32)
                    v_out = wr__[:].rearrange("p k (m two f) -> p (k m) two f", two=2, f=32)
                    ts("dve", v_out[:, :, 0, :], v_in[:, :, 1, :], -1.0, None, ALU.mult, None, [nm], [nm + "r"])
                    cp("dve", v_out[:, :, 1, :], v_in[:, :, 0, :], [nm, nm + "r"], [nm + "r"])
                P.op("dve", lambda e: e.memset(Vp[:, :, :, 128:129], 1.0), writes=["Vp1"])
                for n in range(NT):
                    vb = 4 + (n % 4)
                    for k in range(8):
                        mm(ps[vb][:], hT[:, k, n * 128:(n + 1) * 128], wv[:, k, :], k == 0, k == 7, ["wv", ("hT", n // 4)], [PSR[vb]])
                    cp("act", Vp[:, n, :, 0:128], ps[vb][:].rearrange("p (h c) -> p h c", h=4), [PSR[vb]], ["Vp"])
                for h in range(4):
                    hc = slice(h * 128, (h + 1) * 128)
                    for tb in range(NB):
                        tcols = slice(tb * 512, (tb + 1) * 512)
                        for (w_, wr__, dst, nm, pa, pb) in ((wq, wqr, qT, "qT", 2, 3), (wk, wkr, kT, "kT", 0, 1)):
                            for k in range(8):
                                mm(ps[pa][:], w_[:, k, hc], hT[:, k, tcols], k == 0, k == 7, [nm[0:1] == "q" and "wq" or "wk", ("hT", tb)], [PSR[pa]])
                            for k in range(8):
                                mm(ps[pb][:], wr__[:, k, hc], hT[:, k, tcols], k == 0, k == 7, [(nm[0:1] == "q" and "wq" or "wk") + "r", ("hT", tb)], [PSR[pb]])
                            rta, rtb = (rt1, rt2) if nm == "qT" else (rt3, rt4)
                            tt("dve", rta[:], ropec[:, tcols], ps[pa][:], ALU.mult, ["ropec", PSR[pa]], [nm + "rt1"])
                            tt("dve", rtb[:], ropes[:, tcols], ps[pb][:], ALU.mult, ["ropes", PSR[pb]], [nm + "rt2"])
                            if nm == "qT":
                                tt("dve", dst[:, tcols], rta[:], rtb[:], ALU.add, [nm + "rt1", nm + "rt2"], [nm])
                            else:
                                tt("dve", kTz[0][0:64, tcols], rta[0:64, :], rtb[0:64, :], ALU.add, [nm + "rt1", nm + "rt2"], [nm, "kTz0"])
                                tt("dve", kTz[1][64:128, tcols], rta[64:128, :], rtb[64:128, :], ALU.add, [nm + "rt1", nm + "rt2"], [nm, "kTz1"])
                    def diff_epi_qs(h, hc, qb, m, ab, qs):
                        k = qs % 2
                        sm_, od_, onf_, junk_ = sms[k], ods[k], onfs[k], junks[k]
                        R = lambda nm: (nm, k)
                        bk = ab + qs // 2
                        acc = ps[bk][:, (qs % 2) * 129:(qs % 2) * 129 + 129]
                        P.op("dve", lambda e: e.reciprocal(out=sm_[:, 0:1], in_=acc[:, 128:129]), reads=[PSR[bk]], writes=[R("sm0")])
                        yield
                        if m == 0:
                            ts("dve", o1[:, qs, :], acc[:, 0:128], sm_[:, 0:1], None, ALU.mult, None, [PSR[bk], R("sm0")], [("o1", qs)])
                            yield
                        else:
                            tt("dve", sm_[:, 1:2], sm_[:, 0:1], neglam, ALU.mult, [R("sm0"), "lsc"], [R("sm1")])
                            yield
                            stt(od_[:], acc[:, 0:128], sm_[:, 1:2], o1[:, qs, :], ALU.mult, ALU.add, [PSR[bk], R("sm1"), ("o1", qs)], [R("od")])
                            yield
                            act(junk_[:], od_[:], AF.Square, [R("od")], [R("junk"), R("sm2")], accum_out=sm_[:, 2:3])
                            yield
                            act(sm_[:, 3:4], sm_[:, 2:3], AF.Ln, [R("sm2")], [R("sm3")], scale=1.0 / 128, bias=epsr[:, 0:1])
                            yield
                            act(sm_[:, 4:5], sm_[:, 3:4], AF.Exp, [R("sm3")], [R("sm4")], scale=-0.5)
                            yield
                            stt(onf_[:], od_[:], sm_[:, 4:5], gsc[:, hc], ALU.mult, ALU.mult, [R("od"), R("sm4"), "gsc"], [R("onf")])
                            yield
                            P.op("pe", lambda e: e.transpose(ps[5][:, qs * 128:(qs + 1) * 128], onf_[:], ident[:]),
                                 reads=[R("onf"), "ident"], writes=[PSR[5]])
                            yield

                    def diff_epi(h, hc, qb, m, ab):
                        yield from rr2([diff_epi_qs(h, hc, qb, m, ab, 0), diff_epi_qs(h, hc, qb, m, ab, 1)])
                        yield from rr2([diff_epi_qs(h, hc, qb, m, ab, 2), diff_epi_qs(h, hc, qb, m, ab, 3)])
                        if m == 1:
                            cp("act", yT1[:, h, qb * 512:(qb + 1) * 512], ps[5][:], [PSR[5]], [("yT", 4 + h, qb)])
                            yield

                    for qb in range(NB):
                        for m in range(2):
                            mr = slice(m * 64, (m + 1) * 64)
                            ab = 3 if m == 0 else 6
                            att = attention(lambda cols: qT[:, cols], lambda kt, m=m: kTz[m][:, kt * 128:(kt + 1) * 128],
                                            lambda kt, h=h: Vp[:, kt, h, :], 129, None, 0.125, [0, 1, 2],
                                            lambda qs, ab=ab: (ab + qs // 2, ps[ab + qs // 2][:, (qs % 2) * 129:(qs % 2) * 129 + 129]),
                                            pts, qb, ["qT", "kT", "kTz0", "kTz1", "Vp", "Vp1"], "d")
                            run_unit(att, diff_epi(h, hc, qb, m, ab))
                drain_pending()
                P.flush()
            if stop == "diff":
                P.dma("sp", dbg_d["yT"][:, 0:4, :], yT0[:], "dbg", reads=[])
                P.dma("sp", dbg_d["yT"][:, 4:8, :], yT1[:], "dbg", reads=[])
                P.flush()
                lay.close()
                break

            yT2 = sb("yT2", [128, 4, S], BF16, lay)
            with contextlib.ExitStack() as ph:
                wq = sb("fwq", [128, 8, 512], BF16, ph)
                wk = sb("fwk", [128, 8, 512], BF16, ph)
                wv = sb("fwv", [128, 8, 512], BF16, ph)
                wf = sb("fwf", [128, 8, 8], BF16, ph)
                QA = [sb("QA%d" % i, [128, S], BF16, ph) for i in range(2)]
                KA = [sb("KA%d" % i, [128, S], BF16, ph) for i in range(2)]
                Vp = sb("fVp", [128, NT, 8, 65], BF16, ph)
                pts = [sb("fpt%d" % i, [128, 512], BF16, ph) for i in range(3)]
                fbt = sb("fbt", [8, 2], F32, ph)
                zz = sb("zz", [8, S], F32, ph)
                z2 = sb("z2", [8, S], F32, ph)
                z3 = sb("z3", [8, S], F32, ph)
                chm = sb("chm", [8, 3, S], BF16, ph)
                chn = sb("chn", [8, 3, S], BF16, ph)
                of_ = sb("of_", [128, 4, 128], F32, ph)
                sms = [sb("fsm%d" % i, [128, 8], F32, ph) for i in range(2)]
                P.dma("sp", fbt[:, 0:1], fb_d[l], "ldp", writes=["fbt"])
                wload(wq[:], w_in_v[:, :, C_DV:C_DV + 512], "w0", "fwq")
                wload(wk[:], w_in_v[:, :, C_FQ:C_FQ + 512], "w1", "fwk")
                wload(wv[:], w_in_v[:, :, C_FK:C_FK + 512], "w2", "fwv")
                wload(wf[:], w_in_v[:, :, C_FV:C_FV + 8], "w3", "fwf")
                ts("dve", fbt[:, 1:2], fbt[:, 0:1], -1.0, None, ALU.mult, None, ["fbt"], ["fbt1"])
                for tb in range(NB):
                    tcols = slice(tb * 512, (tb + 1) * 512)
                    for k in range(8):
                        mm(ps[6][0:8, :], wf[:, k, :], hT[:, k, tcols], k == 0, k == 7, ["fwf", ("hT", tb)], [PSR[6]])
                    act(zz[:, tcols], ps[6][0:8, :], AF.Identity, [PSR[6], "fbt1"], ["zz"], scale=-1.0, bias=fbt[:, 1:2])
                ts("dve", z2[:], zz[:], -1.0, None, ALU.mult, None, ["zz"], ["z2"])
                tt("dve", z2[:], z2[:], zz[:], ALU.max, ["z2", "zz"], ["z2"])
                act(z2[:], z2[:], AF.Exp, ["z2"], ["z2"], scale=-1.0)
                act(z2[:], z2[:], AF.Ln, ["z2"], ["z2"], bias=1.0)
                ts("dve", zz[:], zz[:], 0.0, None, ALU.max, None, ["zz"], ["zz"])
                tt("dve", zz[:], zz[:], z2[:], ALU.add, ["zz", "z2"], ["zz"])
                P.op("dve", lambda e: e.memset(z2[:], 1.0), reads=["z2"], writes=["z2"])
                P.op("dve", lambda e: e.tensor_tensor_scan(out=z3[:], data0=z2[:], data1=zz[:], initial=0.0,
                                                           op0=ALU.mult, op1=ALU.add), reads=["z2", "zz"], writes=["z3"])
                ts("dve", chm[:, 0, :], z3[:], -1.0, None, ALU.mult, None, ["z3"], ["chm0"])
                stt(zz[:], z3[:], -1.0, chm[:, 0, :], ALU.mult, ALU.subtract, ["z3", "chm0", "zz"], ["zz"])
                cp("dve", chm[:, 1, :], zz[:], ["zz"], ["chm1"])
                tt("dve", z2[:], zz[:], chm[:, 1, :], ALU.subtract, ["zz", "chm1", "z2"], ["z2"])
                cp("dve", chm[:, 2, :], z2[:], ["z2"], ["chm2"])
                ts("dve", chn[:].rearrange("h r s -> h (r s)"), chm[:].rearrange("h r s -> h (r s)"), -1.0, None, ALU.mult, None,
                   ["chm0", "chm1", "chm2"], ["chn"])
                P.op("dve", lambda e: e.memset(Vp[:, :, :, 64:65], 1.0), writes=["fVp1"])
                for n in range(NT):
                    vb = 2 + (n % 4)
                    for k in range(8):
                        mm(ps[vb][:], hT[:, k, n * 128:(n + 1) * 128], wv[:, k, :], k == 0, k == 7, ["fwv", ("hT", n // 4)], [PSR[vb]])
                    cp("act", Vp[:, n, :, 0:64], ps[vb][:].rearrange("p (h c) -> p h c", h=8), [PSR[vb]], ["fVp"])
                for i in range(2):
                    P.op("dve", lambda e, i=i: e.memset(KA[i][64:128, :], 0.0), writes=[("KA1", i)])
                    P.op("dve", lambda e, i=i: e.memset(QA[i][64:128, :], 0.0), writes=[("QAc", i)])
                    P.op("dve", lambda e, i=i: e.memset(KA[i][64:67, :], 1.0), writes=[("KA1", i)])
                    P.op("dve", lambda e, i=i: e.memset(QA[i][96:99, :], 1.0), writes=[("QAc", i)])
                for hp in range(4):
                    hc = slice(hp * 128, (hp + 1) * 128)
                    for tb in range(NB):
                        tcols = slice(tb * 512, (tb + 1) * 512)
                        for k in range(8):
                            mm(ps[0][:], wq[:, k, hc], hT[:, k, tcols], k == 0, k == 7, ["fwq", ("hT", tb)], [PSR[0]])
                        act(QA[0][0:64, tcols], ps[0][0:64, :], AF.Copy, [PSR[0]], [("QA", 0)], scale=0.125)
                        act(QA[1][0:64, tcols], ps[0][64:128, :], AF.Copy, [PSR[0]], [("QA", 1)], scale=0.125)
                        for k in range(8):
                            mm(ps[1][:], wk[:, k, hc], hT[:, k, tcols], k == 0, k == 7, ["fwk", ("hT", tb)], [PSR[1]])
                        cp("dve", KA[0][0:64, tcols], ps[1][0:64, :], [PSR[1]], [("KA", 0)])
                        cp("dve", KA[1][0:64, tcols], ps[1][64:128, :], [PSR[1]], [("KA", 1)])
                    for i in range(2):
                        h = hp * 2 + i
                        for r3 in range(3):
                            P.dma("sp", QA[i][64 + r3:65 + r3, :], chm[h:h + 1, r3, :], "ldf%d" % i,
                                  reads=["chm0", "chm1", "chm2"], writes=[("QAc", i)])
                            P.dma("sp", KA[i][96 + r3:97 + r3, :], chn[h:h + 1, r3, :], "ldf%d" % i,
                                  reads=["chn"], writes=[("KA1", i)])
                    def fox_epi(hp, qb, i):
                        fb_ = (3, 4, 6, 7)[i + 2 * (qb % 2)]
                        for qs in range(4):
                            k = qs % 2
                            acc = ps[fb_][:, qs * 65:qs * 65 + 65]
                            P.op("dve", lambda e, acc=acc, k=k: e.reciprocal(out=sms[k][:, 0:1], in_=acc[:, 64:65]),
                                 reads=[PSR[fb_]], writes=[("fsm0", k)])
                            yield
                            ts("dve", of_[:, qs, i * 64:(i + 1) * 64], acc[:, 0:64], sms[k][:, 0:1], None, ALU.mult, None,
                               [PSR[fb_], ("fsm0", k)], [("of", qs)])
                            yield
                        if i == 1:
                            for qs in range(4):
                                P.op("pe", lambda e, qs=qs: e.transpose(ps[5][:, qs * 128:(qs + 1) * 128], of_[:, qs, :], ident[:]),
                                     reads=[("of", qs), "ident"], writes=[PSR[5]])
                                yield
                            cp("act", yT2[:, hp, qb * 512:(qb + 1) * 512], ps[5][:], [PSR[5]], [("yT", 8 + hp, qb)])
                            yield

                    for qb in range(NB):
                        for i in range(2):
                            h = hp * 2 + i
                            att = attention(lambda cols, i=i: QA[i][:, cols], lambda kt, i=i: KA[i][:, kt * 128:(kt + 1) * 128],
                                            lambda kt, h=h: Vp[:, kt, h, :], 65, None, 1.0, [0, 1, 2],
                                            lambda qs, i=i, qb=qb: ((3, 4, 6, 7)[i + 2 * (qb % 2)], ps[(3, 4, 6, 7)[i + 2 * (qb % 2)]][:, qs * 65:qs * 65 + 65]),
                                            pts, qb, [("QA", i), ("QAc", i), ("KA", i), ("KA1", i), "fVp", "fVp1"], "f")
                            run_unit(att, fox_epi(hp, qb, i))
                drain_pending()
                P.flush()
            if stop == "fox":
                P.dma("sp", dbg_d["yT"][:, 0:4, :], yT0[:], "dbg", reads=[])
                P.dma("sp", dbg_d["yT"][:, 4:8, :], yT1[:], "dbg", reads=[])
                P.dma("sp", dbg_d["yT"][:, 8:12, :], yT2[:], "dbg", reads=[])
                P.flush()
                lay.close()
                break
            mg = contextlib.ExitStack()
            mergedT = sb("mergedT", [128, 8, S], BF16, mg)
            with contextlib.ExitStack() as ph:
                wg2 = [sb("wg2_%d" % i, [128, 8, 3, 128], BF16, ph) for i in range(2)]
                wb2 = [sb("wb2_%d" % i, [128, 12, 128], BF16, ph) for i in range(2)]
                sgs = [sb("sgs%d" % i, [128, 512], F32, ph) for i in range(3)]
                mt = [sb("mt%d" % i, [128, 512], F32, ph) for i in range(2)]
                wbv = w_br_d[l].rearrange("(c p) d -> p c d", p=128)
                for dc in range(8):
                    s_ = dc % 2
                    for n3 in range(3):
                        c0 = C_FF + n3 * 1024 + dc * 128
                        P.dma("pool", wg2[s_][:, :, n3, :], w_in_v[:, :, c0:c0 + 128], "wg%d" % s_, writes=[("wg2", s_)])
                    P.dma("pool", wb2[s_][:], wbv[:, :, dc * 128:(dc + 1) * 128], "wb%d" % s_, writes=[("wb2", s_)])
                    for tb in range(NB):
                        tcols = slice(tb * 512, (tb + 1) * 512)
                        for n3 in range(3):
                            for k in range(8):
                                mm(ps[n3][:], wg2[s_][:, k, n3, :], hT[:, k, tcols], k == 0, k == 7, [("wg2", s_), ("hT", tb)], [PSR[n3]])
                            for c in range(4):
                                mm(ps[3 + n3][:], wb2[s_][:, n3 * 4 + c, :], (yT0, yT1, yT2)[n3][:, c, tcols], c == 0, c == 3,
                                   [("wb2", s_), ("yT", n3 * 4 + c, tb)], [PSR[3 + n3]])
                            act(sgs[n3][:], ps[n3][:], AF.Sigmoid, [PSR[n3]], [("sgs", n3)])
                        tt("dve", mt[0][:], sgs[0][:], ps[3][:], ALU.mult, [("sgs", 0), PSR[3]], ["mt0"])
                        tt("dve", mt[1][:], sgs[1][:], ps[4][:], ALU.mult, [("sgs", 1), PSR[4]], ["mt1"])
                        tt("dve", mt[0][:], mt[0][:], mt[1][:], ALU.add, ["mt0", "mt1"], ["mt0"])
                        tt("dve", mt[1][:], sgs[2][:], ps[5][:], ALU.mult, [("sgs", 2), PSR[5], "mt1"], ["mt1"])
                        tt("dve", mergedT[:, dc, tcols], mt[0][:], mt[1][:], ALU.add, ["mt0", "mt1"], [("mergedT", tb)])
                P.flush()
            x_src = x_d if l == 0 else x_scr
            with contextlib.ExitStack() as ph:
                wo = sb("wo", [128, 8, D], BF16, ph)
                lng = sb("lng", [128, D], F32, ph)
                lnb = sb("lnb", [128, D], F32, ph)
                xin = [sb("xl%d" % i, [128, D], F32, ph) for i in range(4)]
                zb = [sb("zb%d" % i, [128, D], F32, ph) for i in range(4)]
                xo = [sb("xo%d" % i, [128, D], F32, ph) for i in range(4)]
                stp = dict(st=[sb("lnst%d" % i, [128, 2, 6], F32, ph) for i in range(4)], mv=[sb("lnmv%d" % i, [128, 8], F32, ph) for i in range(4)],
                           xn=[sb("lnxn%d" % i, [128, D], F32, ph) for i in range(4)], eps=sb("lneps", [128, 1], F32, ph))
                P.op("dve", lambda e: e.memset(stp["eps"][:], LN_EPS), writes=["lneps"])
                wov = w_out_d[l].rearrange("(k p) c -> p k c", p=128)
                wload(wo[:, :, 0:512], wov[:, :, 0:512], "w0", "wo")
                wload(wo[:, :, 512:1024], wov[:, :, 512:1024], "w1", "wo")
                P.dma("sp", lng[:], lnrep_d[l, 0], "ldp", writes=["lng"])
                P.dma("sp", lnb[:], lnrep_d[l, 1], "ldp", writes=["lnb"])
                def ln1_tile(n):
                    s_ = n % 4
                    P.dma("sp", xin[s_][:], x_src[n * 128:(n + 1) * 128, :], "lx%d" % s_, writes=[("xin", s_)])
                    for hf in range(2):
                        b = 4 + hf + 2 * (s_ % 2)
                        for dc in range(8):
                            mm(ps[b][:], mergedT[:, dc, n * 128:(n + 1) * 128], wo[:, dc, hf * 512:(hf + 1) * 512], dc == 0, dc == 7,
                               ["wo", ("mergedT", n // 4)], [PSR[b]])
                        stt(zb[s_][:, hf * 512:(hf + 1) * 512], xin[s_][:, hf * 512:(hf + 1) * 512], ALPHA, ps[b][:], ALU.mult, ALU.add,
                            [("xin", s_), PSR[b]], [("zb", s_)])
                        yield
                    yield from layer_norm(xo[s_][:], ("xo", s_), zb[s_][:], ("zb", s_), lng[:], lnb[:], stp, s_)
                    P.dma("sp", x_scr[n * 128:(n + 1) * 128, :], xo[s_][:], "sx%d" % s_, reads=[("xo", s_)])
                    transposes(n, [0, 1, 2, 3], xo[s_], ("xo", s_))
                    yield
                for n in range(0, NT, 4):
                    interleave([ln1_tile(n + i) for i in range(4)])
                P.flush()
            mg.close()
            lay.close()
            if stop == "ln1":
                with contextlib.ExitStack() as ph:
                    xt = sb("xt", [128, NT, D], F32, ph)
                    P.dma("sp", xt[:], x_scr.rearrange("(n p) d -> p n d", p=128), "dbg", writes=["xt"])
                    P.dma("sp", dbg_d["x1"], xt[:], "dbg", reads=["xt"])
                    P.flush()
                break
            with contextlib.ExitStack() as ph:
                x_sb = sb("x_sb", [128, NT, D], F32, ph)
                hid = sb("hid", [128, 8, S], BF16, ph)
                wgu = [sb("wgu%d" % i, [128, 8, 512], BF16, ph) for i in range(2)]
                wdn = [sb("wdn%d" % i, [128, 2, D], BF16, ph) for i in range(4)]
                wr = sb("wr", [128, 8, 20], BF16, ph)
                brp = sb("brp", [128, 20], F32, ph)
                sel = sb("sel", [128, 16, 128], BF16, ph)
                gTb = sb("gTb", [128, S], BF16, ph)
                lg = sb("lg", [128, NT, 20], F32, ph)
                rg = sb("rg", [128, 12, NT], F32, ph)
                r4 = [sb("r4_%d" % i, [128, NT, 4], F32, ph) for i in range(6)]
                r16 = sb("r16", [128, NT, 4, 4], F32, ph)
                gate = sb("gate", [128, NT, 16], F32, ph)
                gateT = sb("gateT", [32, S], F32, ph)
                gbs = [sb("gbs%d" % i, [128, 512], F32, ph) for i in range(2)]
                slbuf = sb("slbuf", [128, 4, 512], F32, ph)
                sl = [[slbuf[:, i * 2 + p_, :] for p_ in range(2)] for i in range(2)]
                lng = sb("lng2", [128, D], F32, ph)
                lnb = sb("lnb2", [128, D], F32, ph)
                class _V:
                    def __init__(self, ap):
                        self.ap = ap

                    def __getitem__(self, key):
                        return self.ap
                hidf = hid[:].rearrange("p a s -> p (a s)").bitcast(F32)
                stp = dict(st=[sb("lnst2%d" % i, [128, 2, 6], F32, ph) for i in range(4)], mv=[sb("lnmv2%d" % i, [128, 8], F32, ph) for i in range(4)],
                           xn=[_V(slbuf[:, 2 * k:2 * k + 2, :].rearrange("p a c -> p (a c)")) for k in range(2)] +
                              [_V(hidf[:, k * 1024:(k + 1) * 1024]) for k in range(2)],
                           eps=sb("lneps2", [128, 1], F32, ph))
                P.op("dve", lambda e: e.memset(stp["eps"][:], LN_EPS), writes=["lneps"])
                P.dma("sp", x_sb[:], x_scr.rearrange("(n p) d -> p n d", p=128), "ldx", writes=[("x", n) for n in range(NT)])
                P.dma("sp", lng[:], lnrep_d[l, 2], "ldp", writes=["lng"])
                P.dma("sp", lnb[:], lnrep_d[l, 3], "ldp", writes=["lnb"])
                P.dma("sp", brp[:], brep_d[l], "ldp", writes=["brp"])
                wload(sel[:], sel_d, "w1", "sel")
                P.op("dve", lambda e: e.memset(gTb[:], 0.0), writes=["gTb"])
                wload(wr[:], wr_d[l].rearrange("(k p) c -> p k c", p=128), "w0", "wr")
                lgT = gateT
                for tb in range(NB):
                    tcols = slice(tb * 512, (tb + 1) * 512)
                    for k in range(8):
                        mm(ps[6][0:20, :], wr[:, k, :], hT[:, k, tcols], k == 0, k == 7, ["wr", ("hT", tb)], [PSR[6]])
                    cp("act", lgT[0:20, tcols], ps[6][0:20, :], [PSR[6]], ["gateT"])
                for n in range(NT):
                    b = 6 + n // 8
                    P.op("pe", lambda e, n=n, b=b: e.matmul(ps[b][:, (n % 8) * 64:(n % 8) * 64 + 64], lhsT=lgT[0:20, n * 128:(n + 1) * 128],
                                                            rhs=ident[0:20, 0:64], start=True, stop=True, skip_group_check=True),
                         reads=["gateT", "ident"], writes=[PSR[b]])
                for hb in range(2):
                    tt("dve", lg[:, hb * 8:(hb + 1) * 8, :], ps[6 + hb][:].rearrange("p (n c) -> p n c", n=8)[:, :, 0:20],
                       brp[:].unsqueeze(1).to_broadcast([128, 8, 20]), ALU.add, [PSR[6 + hb], "brp"], ["lg"])
                LG = lg[:, :, 0:4]
                LE = lg[:, :, 4:20].rearrange("p n (g e) -> p n g e", g=4)
                bc4 = lambda ap: ap.unsqueeze(2).to_broadcast([128, NT, 4])
                RG = lambda i: rg[:, i, :]
                P.op("dve", lambda e: e.reduce_max(out=RG(0), in_=LG, axis=AX.X), reads=["lg"], writes=["rg0"])
                tt("dve", r4[0][:], LG, bc4(RG(0)), ALU.subtract, ["lg", "rg0"], ["r4_0"])
                act(r4[0][:], r4[0][:], AF.Exp, ["r4_0"], ["r4_0"])
                P.op("dve", lambda e: e.reduce_sum(out=RG(1), in_=r4[0][:], axis=AX.X), reads=["r4_0"], writes=["rg1"])
                P.op("dve", lambda e: e.reciprocal(out=RG(1), in_=RG(1)), reads=["rg1"], writes=["rg1"])
                tt("dve", r4[1][:], LG, bc4(RG(0)), ALU.is_equal, ["lg", "rg0"], ["r4_1"])
                tt("dve", r16[:], LE, r4[1][:].unsqueeze(3).to_broadcast([128, NT, 4, 4]), ALU.mult, ["lg", "r4_1"], ["r16"])
                P.op("dve", lambda e: e.tensor_reduce(out=r4[2][:], in_=r16[:].rearrange("p n g e -> p n e g"), axis=AX.X, op=ALU.add),
                     reads=["r16"], writes=["r4_2"])
                P.op("dve", lambda e: e.reduce_max(out=RG(2), in_=r4[2][:], axis=AX.X), reads=["r4_2"], writes=["rg2"])
                tt("dve", r4[3][:], r4[2][:], bc4(RG(2)), ALU.is_equal, ["r4_2", "rg2"], ["r4_3"])
                stt(r4[4][:], r4[3][:], -1e30, r4[2][:], ALU.mult, ALU.add, ["r4_3", "r4_2"], ["r4_4"])
                P.op("dve", lambda e: e.reduce_max(out=RG(3), in_=r4[4][:], axis=AX.X), reads=["r4_4"], writes=["rg3"])
                tt("dve", r4[5][:], r4[4][:], bc4(RG(3)), ALU.is_equal, ["r4_4", "rg3"], ["r4_5"])
                tt("dve", RG(4), RG(3), RG(2), ALU.subtract, ["rg3", "rg2"], ["rg4"])
                act(RG(4), RG(4), AF.Exp, ["rg4"], ["rg4"])
                ts("dve", RG(5), RG(4), 1.0, None, ALU.add, None, ["rg4"], ["rg5"])
                P.op("dve", lambda e: e.reciprocal(out=RG(5), in_=RG(5)), reads=["rg5"], writes=["rg5"])
                tt("dve", RG(6), RG(4), RG(5), ALU.mult, ["rg4", "rg5"], ["rg6"])
                tt("dve", RG(5), RG(5), RG(1), ALU.mult, ["rg5", "rg1"], ["rg5"])
                tt("dve", RG(6), RG(6), RG(1), ALU.mult, ["rg6", "rg1"], ["rg6"])
                tt("dve", r4[3][:], r4[3][:], bc4(RG(5)), ALU.mult, ["r4_3", "rg5"], ["r4_3"])
                tt("dve", r4[5][:], r4[5][:], bc4(RG(6)), ALU.mult, ["r4_5", "rg6"], ["r4_5"])
                tt("dve", r4[3][:], r4[3][:], r4[5][:], ALU.add, ["r4_3", "r4_5"], ["r4_3"])
                tt("dve", gate[:].rearrange("p n (g e) -> p n g e", g=4), r4[1][:].unsqueeze(3).to_broadcast([128, NT, 4, 4]),
                   r4[3][:].unsqueeze(2).to_broadcast([128, NT, 4, 4]), ALU.mult, ["r4_1", "r4_3"], ["gate"])
                for n in range(NT):
                    b = 4 + (n // 4) % 2
                    P.op("pe", lambda e, n=n, b=b: e.transpose(ps[b][0:16, (n % 4) * 128:(n % 4 + 1) * 128], gate[:, n, :], ident[:]),
                         reads=["gate", "ident"], writes=[PSR[b]])
                    if n % 4 == 3:
                        gcols = slice((n // 4) * 512, (n // 4 + 1) * 512)
                        cp("act", gateT[0:16, gcols], ps[b][0:16, :], [PSR[b]], ["gateT"])
                        cp("dve", gTb[0:16, gcols], gateT[0:16, gcols], ["gateT"], ["gTb"])
                        tt("dve", gTb[32:48, gcols], gateT[0:16, gcols], gTb[0:16, gcols], ALU.subtract, ["gateT", "gTb"], ["gTb"])
                if "gate" in dbg_d:
                    P.dma("sp", dbg_d["gate"], gate[:], "dbg", reads=["gate"])
                if "lg" in dbg_d:
                    P.dma("sp", dbg_d["lg"], lg[:], "dbg", reads=["lg"])
                if "ohg" in dbg_d:
                    P.dma("sp", dbg_d["ohg"], r4[1][:], "dbg", reads=["r4_1"])
                    P.dma("sp", dbg_d["elsel"], r4[2][:], "dbg", reads=["r4_2"])
                    P.dma("sp", dbg_d["rg"], rg[:], "dbg", reads=["rg%d" % i for i in range(7)])
                for n in range(NT):
                    act(x_sb[:, n, :], x_sb[:, n, :], AF.Copy, [("x", n)], [("x", n)], scale=ALPHA)
                for G in range(4):
                    for ee in range(4):
                        e_ = G * 4 + ee
                        s_ = e_ % 2
                        P.dma("pool", wgu[s_][:, :, 0:256], wg_d[l, e_].rearrange("(k p) c -> p k c", p=128), "wgu%d" % s_, writes=[("wgu", s_)])
                        P.dma("pool", wgu[s_][:, :, 256:512], wu_d[l, e_].rearrange("(k p) c -> p k c", p=128), "wgu%d" % s_, writes=[("wgu", s_)])
                        P.dma("pool", wdn[ee][:], wd_d[l, e_].rearrange("(c p) d -> p c d", p=128), "wdn%d" % ee, writes=[("wdn", ee)])
                        for tb in range(NB):
                            tcols = slice(tb * 512, (tb + 1) * 512)
                            def proj(cb):
                                for k in range(8):
                                    mm(ps[cb][:], wgu[s_][:, k, cb * 128:(cb + 1) * 128], hT[:, k, tcols], k == 0, k == 7,
                                       [("wgu", s_), ("hT", tb)], [PSR[cb]])
                            pp = tb % 2
                            proj(0)
                            act(sl[0][pp], ps[0][:], AF.Silu, [PSR[0]], [("sl", 0, pp)])
                            mm(ps[4][:], sel[:, e_, :], gTb[:, tcols], True, True, ["sel", "gTb"], [PSR[4]])
                            cp("act", gbs[pp][:], ps[4][:], [PSR[4]], [("gbs", pp)])
                            proj(1)
                            act(sl[1][pp], ps[1][:], AF.Silu, [PSR[1]], [("sl", 1, pp)])
                            for fc in range(2):
                                proj(2 + fc)
                                tt("dve", sl[fc][pp], sl[fc][pp], ps[2 + fc][:], ALU.mult, [("sl", fc, pp), PSR[2 + fc]], [("sl", fc, pp)])
                                tt("dve", hid[:, ee * 2 + fc, tcols], sl[fc][pp], gbs[pp][:], ALU.mult, [("sl", fc, pp), ("gbs", pp)], [("hid", tb)])
                    def stage_b(n):
                        for hf in range(2):
                            b = 5 + (2 * n + hf) % 3
                            for i8 in range(8):
                                mm(ps[b][:], hid[:, i8, n * 128:(n + 1) * 128], wdn[i8 // 2][:, i8 % 2, hf * 512:(hf + 1) * 512],
                                   i8 == 0, i8 == 7, [("hid", n // 4), ("wdn", i8 // 2)], [PSR[b]])
                            tt("dve", x_sb[:, n, hf * 512:(hf + 1) * 512], x_sb[:, n, hf * 512:(hf + 1) * 512], ps[b][:], ALU.add,
                               [("x", n), PSR[b]], [("x", n)])
                    if G < 3:
                        for n in range(NT):
                            stage_b(n)
                    else:
                        last = (l == NL - 1)

                        def ln2_a(n):
                            yield from layer_norm(x_sb[:, n, :], ("x", n), x_sb[:, n, :], ("x", n), lng[:], lnb[:], stp, n % 4, inplace=True)

                        def ln2_b(n):
                            if last:
                                P.dma("sp", out_d[n * 128:(n + 1) * 128, :], x_sb[:, n, :], "so%d" % (n % 2), reads=[("x", n)])
                            else:
                                P.dma("sp", x_scr[n * 128:(n + 1) * 128, :], x_sb[:, n, :], "so%d" % (n % 2), reads=[("x", n)])
                                transposes(n, [0, 1, 2, 3], x_sb[:, n, :], ("x", n))
                        for g4 in range(4):
                            for n in range(g4 * 4, g4 * 4 + 4):
                                stage_b(n)
                            if g4 >= 1:
                                for n in range((g4 - 1) * 4, g4 * 4):
                                    ln2_b(n)
                            interleave([ln2_a(n) for n in range(g4 * 4, g4 * 4 + 4)])
                        for n in range(12, 16):
                            ln2_b(n)
                P.flush()

        P.flush(final=True)
    return nc


def prep_shared(inp, S=2048):
    f = np.float32
    L = NLT
    G, Pn, N = 32, 64, 16
    out = {}
    out["w_in"] = np.ascontiguousarray(inp["w_in"], f)
    out["w_branch"] = np.ascontiguousarray(inp["w_branch"], f).reshape(L, 3 * 512, D)
    out["w_out"] = np.ascontiguousarray(inp["w_out"], f)
    lr, li, ldt = inp["ssm_lambda_re"], inp["ssm_lambda_im"], inp["ssm_log_dt"]
    ldt_full = np.broadcast_to(ldt[:, :, None], (L, G, Pn))
    stack = np.stack([lr, li, ldt_full], axis=-1).reshape(L, G * Pn, 3)
    out["lampq"] = np.ascontiguousarray(stack.reshape(L, 16, 128, 3).transpose(0, 2, 1, 3), f)
    rep = np.stack([lr.reshape(L, -1), li.reshape(L, -1), ldt_full.reshape(L, -1)], axis=1)
    out["lamrep"] = np.ascontiguousarray(np.broadcast_to(rep[:, None], (L, 128, 3, 2048)), f)
    def bblk(b):
        o = np.zeros((L, 128, 16, 128), f)
        for j in range(16):
            for g2 in range(2):
                g = 2 * j + g2
                gl = g % 8
                o[:, gl * 16:(gl + 1) * 16, j, g2 * 64:(g2 + 1) * 64] = b[:, g].transpose(0, 2, 1)
        return o
    def cblk(c):
        o = np.zeros((L, 128, 16, 128), f)
        for j in range(16):
            for g2 in range(2):
                g = 2 * j + g2
                gl = g % 8
                o[:, g2 * 64:(g2 + 1) * 64, j, gl * 16:(gl + 1) * 16] = c[:, g].transpose(0, 2, 1)
        return o
    out["bblk_re"] = bblk(inp["ssm_b_re"])
    out["bblk_im"] = bblk(inp["ssm_b_im"])
    out["bblkT_re"] = cblk(np.ascontiguousarray(inp["ssm_b_re"].transpose(0, 1, 3, 2)))
    out["bblkT_im"] = cblk(np.ascontiguousarray(inp["ssm_b_im"].transpose(0, 1, 3, 2)))
    out["cblk_re"] = cblk(inp["ssm_c_re"])
    out["cblk_im"] = cblk(inp["ssm_c_im"])
    out["dpq"] = np.ascontiguousarray(inp["ssm_d"].reshape(L, 4, 128).transpose(0, 2, 1), f)
    out["w_glu"] = np.ascontiguousarray(inp["ssm_w_glu"], f)
    out["dlam"] = np.ascontiguousarray(np.broadcast_to(inp["diff_lambda"][:, None], (L, 128, 4, 64)), f)
    out["dgrep"] = np.ascontiguousarray(np.broadcast_to(inp["diff_norm_g"][:, None], (L, 128, 512)), f)
    out["fb"] = np.ascontiguousarray(inp["fox_f_bias"].reshape(L, 8, 1), f)
    lnst = np.stack([inp["ln1_g"], inp["ln1_b"], inp["ln2_g"], inp["ln2_b"]], axis=1)
    out["lnrep"] = np.ascontiguousarray(np.broadcast_to(lnst[:, :, None], (L, 4, 128, D)), f)
    out["w_r"] = np.ascontiguousarray(np.concatenate([inp["moe_w_group"], inp["moe_w_expert"]], axis=-1), f)
    br = np.concatenate([inp["moe_b_group"], inp["moe_b_expert"]], axis=-1)
    out["b_rrep"] = np.ascontiguousarray(np.broadcast_to(br[:, None], (L, 128, 20)), f)
    out["moe_w_gate"] = np.ascontiguousarray(inp["moe_w_gate"], f)
    out["moe_w_up"] = np.ascontiguousarray(inp["moe_w_up"], f)
    out["moe_w_down"] = np.ascontiguousarray(inp["moe_w_down"], f)
    pos = np.arange(S, dtype=np.float32)
    inv_freq = (10000.0 ** (-np.arange(0, 64, 2, dtype=np.float32) / 64)).astype(np.float32)
    ang = pos[None, :] * inv_freq[np.arange(128) % 32][:, None]
    out["ropec"] = np.cos(ang).astype(f)
    out["ropes"] = np.sin(ang).astype(f)
    sel = np.zeros((128, 16, 128), f)
    for e in range(16):
        sel[e, e, :] = 1.0
        sel[32 + e, e, :] = 1.0
    out["sel"] = sel
    return out


_NC_CACHE = {}


def kernel(**inputs):
    S = 2048
    shared = prep_shared(inputs, S)
    x = np.ascontiguousarray(inputs["x"], np.float32)
    if "nc" not in _NC_CACHE:
        _NC_CACHE["nc"] = build(NL=4, S=S)
    nc = _NC_CACHE["nc"]
    in_maps = []
    for b in range(8):
        m = dict(shared)
        m["x"] = x[b]
        in_maps.append(m)
    res = run_bass_kernel_spmd(nc, in_maps, core_ids=list(range(8)))
    return np.stack([np.asarray(r["out"], np.float32) for r in res.results], axis=0)
```

```python
import math
import contextlib
import numpy as np
import concourse.bass as bass
import concourse.mybir as mybir
from concourse.bass_utils import run_bass_kernel_spmd

F32 = mybir.dt.float32
BF16 = mybir.dt.bfloat16
I32 = mybir.dt.int32
AF = mybir.ActivationFunctionType
ALU = mybir.AluOpType
AX = mybir.AxisListType

D = 1024
NLT = 4
D_IN = 6664
C_SSM, C_DQ, C_DK, C_DV, C_FQ, C_FK, C_FV, C_FF = 512, 1024, 1536, 2048, 2560, 3072, 3584, 3592
ALPHA = 8 ** 0.25
LN_EPS = 1e-5
RMS_EPS = 1e-6
TWO_PI = 2.0 * math.pi
ENGS = ("pe", "act", "dve", "pool", "sp")
SAME_ENG_SYNC = True
FUSE_WAIT = True


class H:
    __slots__ = ("eng", "idx", "dma_sem", "dma_val")

    def __init__(self, eng, idx, dma_sem=None, dma_val=None):
        self.eng, self.idx, self.dma_sem, self.dma_val = eng, idx, dma_sem, dma_val


class Prog:
    def __init__(self, nc, st):
        self.nc, self.st = nc, st
        self.ops = {e: [] for e in ENGS}
        self.base = {e: 0 for e in ENGS}
        self.cnt = {e: 0 for e in ENGS}
        self.esem = {e: st.enter_context(nc.semaphore("s_" + e)) for e in ENGS}
        self.dsem = {}
        NS = 32
        for k in range(NS):
            self.dsem[k] = [st.enter_context(nc.semaphore("d_%d" % k)), 0]
        self.ring_next = 0
        self.NS = NS
        self.lastw = {}
        self.readers = {}
        self.waited = {e: {} for e in ENGS}
        self.sigval = {}
        self.nphase = 0

    def _deps(self, reads, writes):
        deps = []
        for r in reads:
            w = self.lastw.get(r)
            if w is not None:
                deps.append(w)
        for r in writes:
            w = self.lastw.get(r)
            if w is not None:
                deps.append(w)
            deps.extend(self.readers.get(r, {}).values())
        return deps

    def _update(self, h, reads, writes):
        key = h.eng if h.dma_sem is None else ("dma", h.dma_sem)
        for r in reads:
            self.readers.setdefault(r, {})[key] = h
        for r in writes:
            self.lastw[r] = h
            self.readers[r] = {}

    def op(self, eng, fn, reads=(), writes=()):
        px = [r for r in reads if isinstance(r, str) and r.startswith("ps")]
        if px:
            reads = [r for r in reads if r not in px]
            writes = list(writes) + px
        deps = self._deps(reads, writes)
        lst = self.ops[eng]
        lst.append(dict(fn=fn, deps=deps, sig=False, dma=None))
        h = H(eng, self.base[eng] + len(lst) - 1)
        self._update(h, reads, writes)
        return h

    def dma(self, eng, out, in_, sem, reads=(), writes=(), **kw):
        k = self.ring_next
        self.ring_next = (k + 1) % self.NS
        prev = self.dsem[k][1]
        self.dsem[k][1] += 16
        val = self.dsem[k][1]
        deps = self._deps(reads, writes)
        if prev > 0:
            deps.append(H(eng, -1, dma_sem=k, dma_val=prev))
        lst = self.ops[eng]
        lst.append(dict(fn=lambda e: e.dma_start(out=out, in_=in_, **kw), deps=deps, sig=False, dma=k))
        h = H(eng, self.base[eng] + len(lst) - 1, dma_sem=k, dma_val=val)
        self._update(h, reads, writes)
        return h

    def flush(self, final=False):
        nc = self.nc
        for e in ENGS:
            for o in self.ops[e]:
                for d in o["deps"]:
                    if d.dma_sem is None and d.idx >= self.base[d.eng]:
                        if d.eng == "pe" and e == "pe":
                            continue
                        if d.eng == e and not SAME_ENG_SYNC:
                            continue
                        self.ops[d.eng][d.idx - self.base[d.eng]]["sig"] = True
        for e in ENGS:
            c = self.cnt[e]
            for i, o in enumerate(self.ops[e]):
                if o["sig"]:
                    c += 1
                self.sigval[(e, self.base[e] + i)] = c
            self.cnt[e] = c
        used_dma = set(o["dma"] for e in ENGS for o in self.ops[e] if o["dma"] is not None)
        self.nphase += 1
        with nc.Block(no_gpsimd_drain=(not final)) as block:
            def run(ename):
                def body(eng):
                    waited = self.waited[ename]
                    for i, o in enumerate(self.ops[ename]):
                        pend = []
                        for d in o["deps"]:
                            if d.dma_sem is not None:
                                key, val, sem = ("dma", d.dma_sem), d.dma_val, self.dsem[d.dma_sem][0]
                            else:
                                if d.idx < self.base[d.eng]:
                                    continue
                                if d.eng == "pe" and ename == "pe":
                                    continue
                                if d.eng == ename and not SAME_ENG_SYNC:
                                    continue
                                key, val, sem = d.eng, self.sigval[(d.eng, d.idx)], self.esem[d.eng]
                            if waited.get(key, 0) >= val:
                                continue
                            waited[key] = val
                            pend.append((sem, val))
                        fuse = FUSE_WAIT and o["dma"] is None and len(pend) > 0
                        for (sem, val) in (pend[:-1] if fuse else pend):
                            eng.wait_ge(sem, val)
                        ins = o["fn"](eng)
                        if fuse:
                            ins._wait_ge(pend[-1][0], pend[-1][1])
                        if o["dma"] is not None:
                            ins.then_inc(self.dsem[o["dma"]][0], 16)
                        elif o["sig"]:
                            ins.then_inc(self.esem[ename], 1)
                    if ename == "sp":
                        for name in sorted(used_dma):
                            v = self.dsem[name][1]
                            if waited.get(("dma", name), 0) < v:
                                waited[("dma", name)] = v
                                eng.wait_ge(self.dsem[name][0], v)
                return body

            block.tensor(run("pe"))
            block.scalar(run("act"))
            block.vector(run("dve"))
            block.gpsimd(run("pool"))
            block.sync(run("sp"))
        for e in ENGS:
            self.base[e] += len(self.ops[e])
            self.ops[e] = []
        self.lastw = {}
        self.readers = {}


def build(NL=4, S=2048, dbg=(), stop=None):
    NT, NB = S // 128, S // 512
    nc = bass.Bass("TRN2", target_bir_lowering=False)

    def din(name, shape):
        return nc.dram_tensor(name, list(shape), F32, kind="ExternalInput").ap()

    x_d = din("x", [S, D])
    w_in_d = din("w_in", [NLT, D, D_IN])
    w_br_d = din("w_branch", [NLT, 3 * 512, D])
    w_out_d = din("w_out", [NLT, D, D])
    lampq_d = din("lampq", [NLT, 128, 16, 3])
    lamrep_d = din("lamrep", [NLT, 128, 3, 2048])
    bblk_re_d = din("bblk_re", [NLT, 128, 16, 128])
    bblk_im_d = din("bblk_im", [NLT, 128, 16, 128])
    bblkT_re_d = din("bblkT_re", [NLT, 128, 16, 128])
    bblkT_im_d = din("bblkT_im", [NLT, 128, 16, 128])
    cblk_re_d = din("cblk_re", [NLT, 128, 16, 128])
    cblk_im_d = din("cblk_im", [NLT, 128, 16, 128])
    dpq_d = din("dpq", [NLT, 128, 4])
    wglu_d = din("w_glu", [NLT, 512, 1024])
    dlam_d = din("dlam", [NLT, 128, 4, 64])
    dg_d = din("dgrep", [NLT, 128, 512])
    fb_d = din("fb", [NLT, 8, 1])
    lnrep_d = din("lnrep", [NLT, 4, 128, D])
    wr_d = din("w_r", [NLT, D, 20])
    brep_d = din("b_rrep", [NLT, 128, 20])
    wg_d = din("moe_w_gate", [NLT, 16, D, 256])
    wu_d = din("moe_w_up", [NLT, 16, D, 256])
    wd_d = din("moe_w_down", [NLT, 16, 256, D])
    ropec_d = din("ropec", [128, S])
    ropes_d = din("ropes", [128, S])
    sel_d = din("sel", [128, 16, 128])
    out_d = nc.dram_tensor("out", [S, D], F32, kind="ExternalOutput").ap()
    dbg_d = {}
    for name, shape, dt_ in dbg:
        dbg_d[name] = nc.dram_tensor("dbg_" + name, list(shape), dt_, kind="ExternalOutput").ap()

    with contextlib.ExitStack() as st:
        uid = [0]

        def sb(name, shape, dt, stack=st):
            uid[0] += 1
            return stack.enter_context(nc.sbuf_tensor("sb%d_%s" % (uid[0], name), list(shape), dt))

        P = Prog(nc, st)
        ps = [st.enter_context(nc.psum_tensor("ps%d" % i, [128, 512], F32)) for i in range(8)]
        PSR = ["ps%d" % i for i in range(8)]

        x_scr = nc.dram_tensor("x_scr", [S, D], F32, kind="Internal").ap()
        hT = sb("hT", [128, 8, S], BF16)
        ident = sb("ident", [128, 128], F32)
        tri = sb("tri", [128, 128], BF16)
        iota_i = sb("iota_i", [128, 257], I32)
        iota_f = sb("iota_f", [128, 257], F32)

        def mm(out, lhsT, rhs, start, stop, reads, writes):
            return P.op("pe", lambda e: e.matmul(out, lhsT=lhsT, rhs=rhs, start=start, stop=stop,
                                                  skip_group_check=True), reads=reads, writes=writes)

        def act(out, in_, func, reads, writes, **kw):
            return P.op("act", lambda e: e.activation(out=out, in_=in_, func=func, **kw), reads=reads, writes=writes)

        def tt(eng, out, in0, in1, op, reads, writes):
            return P.op(eng, lambda e: e.tensor_tensor(out=out, in0=in0, in1=in1, op=op), reads=reads, writes=writes)

        def ts(eng, out, in0, s1, s2, op0, op1, reads, writes):
            if s2 is None:
                return P.op(eng, lambda e: e.tensor_scalar(out=out, in0=in0, scalar1=s1, scalar2=None, op0=op0),
                            reads=reads, writes=writes)
            return P.op(eng, lambda e: e.tensor_scalar(out=out, in0=in0, scalar1=s1, scalar2=s2, op0=op0, op1=op1),
                        reads=reads, writes=writes)

        def stt(out, in0, scalar, in1, op0, op1, reads, writes):
            return P.op("dve", lambda e: e.scalar_tensor_tensor(out=out, in0=in0, scalar=scalar, in1=in1, op0=op0, op1=op1),
                        reads=reads, writes=writes)

        def cp(eng, out, in_, reads, writes):
            if eng == "act":
                return act(out, in_, AF.Copy, reads, writes)
            return P.op(eng, lambda e: e.tensor_copy(out=out, in_=in_), reads=reads, writes=writes)

        def wload(dst, src, sem, res, eng="pool"):
            return P.dma(eng, dst, src, sem, writes=[res])

        def dump(name, src_ap, reads):
            if name in dbg_d:
                P.dma("sp", dbg_d[name], src_ap, "dbg", reads=reads)

        tr_rr = [0]

        def transposes(n, banks, src, src_res):
            for hb in range(2):
                b = banks[tr_rr[0] % len(banks)]
                tr_rr[0] += 1
                for i in range(4):
                    c = hb * 4 + i
                    P.op("pe", lambda e, b=b, i=i, c=c: e.transpose(ps[b][:, i * 128:(i + 1) * 128],
                                                                     src[:, c * 128:(c + 1) * 128], ident[:]),
                         reads=[src_res, "ident"], writes=[PSR[b]])
                cp("act", hT[:, hb * 4:(hb + 1) * 4, n * 128:(n + 1) * 128],
                   ps[b][:].rearrange("p (c t) -> p c t", c=4), reads=[PSR[b]], writes=[("hT", n // 4)])

        def interleave(gens):
            gens = list(gens)
            while gens:
                for g in list(gens):
                    try:
                        next(g)
                    except StopIteration:
                        gens.remove(g)

        def layer_norm(dst_ap, dst_res, z_ap, z_res, grep, brep, stp, k, extra=(), inplace=False):
            stt_ = stp["st"][k]
            mv = stp["mv"][k]
            xn = None if inplace else stp["xn"][k]
            R = lambda nm: (nm, k)
            for i in range(2):
                P.op("dve", lambda e, i=i: e.bn_stats(out=stt_[:, i, :], in_=z_ap[:, i * 512:(i + 1) * 512]),
                     reads=[z_res], writes=[R("lnst")])
                yield
            P.op("dve", lambda e: e.bn_aggr(out=mv[:, 0:2], in_=stt_[:].rearrange("p a b -> p (a b)")),
                 reads=[R("lnst")], writes=[R("lnmv")])
            yield
            act(mv[:, 2:3], mv[:, 1:2], AF.Ln, [R("lnmv")], [R("lnmv2")], bias=stp["eps"][:, 0:1])
            yield
            act(mv[:, 3:4], mv[:, 2:3], AF.Exp, [R("lnmv2")], [R("lnmv3")], scale=-0.5)
            yield
            stt(mv[:, 4:5], mv[:, 0:1], -1.0, mv[:, 3:4], ALU.mult, ALU.mult, [R("lnmv"), R("lnmv3")], [R("lnmv4")])
            yield
            if inplace:
                act(dst_ap, z_ap, AF.Identity, [z_res, R("lnmv3"), R("lnmv4")], [dst_res], scale=mv[:, 3:4], bias=mv[:, 4:5])
                yield
                tt("dve", dst_ap, dst_ap, grep, ALU.mult, [dst_res, "lng"], [dst_res])
                yield
                tt("dve", dst_ap, dst_ap, brep, ALU.add, [dst_res, "lnb"], [dst_res])
                yield
                return
            act(xn[:], z_ap, AF.Identity, [z_res, R("lnmv3"), R("lnmv4")], [R("lnxn")] + list(extra), scale=mv[:, 3:4], bias=mv[:, 4:5])
            yield
            tt("dve", xn[:], xn[:], grep, ALU.mult, [R("lnxn"), "lng"], [R("lnxn")])
            yield
            tt("dve", dst_ap, xn[:], brep, ALU.add, [R("lnxn"), "lnb", z_res], [dst_res])
            yield

        P.op("pool", lambda e: e.memset(ident[:], 1.0), writes=["ident"])
        P.op("pool", lambda e: e.affine_select(out=ident[:], in_=ident[:], pattern=[[-1, 128]], base=0,
                                               channel_multiplier=1, compare_op=ALU.is_equal, fill=0.0),
             reads=["ident"], writes=["ident"])
        P.op("pool", lambda e: e.memset(tri[:], 1.0), writes=["tri"])
        P.op("pool", lambda e: e.affine_select(out=tri[:], in_=tri[:], pattern=[[1, 128]], base=0,
                                               channel_multiplier=-1, compare_op=ALU.is_ge, fill=0.0),
             reads=["tri"], writes=["tri"])
        P.op("pool", lambda e: e.iota(iota_i[:], pattern=[[1, 257]], base=0, channel_multiplier=0), writes=["iota_i"])
        cp("dve", iota_f[:], iota_i[:], ["iota_i"], ["iota_f"])
        with contextlib.ExitStack() as ph:
            xin = [sb("xin%d" % i, [128, D], F32, ph) for i in range(3)]
            for n in range(NT):
                P.dma("sp", xin[n % 3][:], x_d[n * 128:(n + 1) * 128, :], "ldx%d" % (n % 3), writes=[("xin", n % 3)])
                transposes(n, [0, 1, 2, 3], xin[n % 3], ("xin", n % 3))
            P.flush()

        for l in range(NL):
            lam_init = 0.8 - 0.6 * math.exp(-0.3 * l)
            w_in_v = w_in_d[l].rearrange("(k p) c -> p k c", p=128)

            lay = contextlib.ExitStack()
            yT0 = sb("yT0", [128, 4, S], BF16, lay)
            with contextlib.ExitStack() as ph:
                NC_ = 128
                lpq = sb("lpq", [128, 16, 3], F32, ph)
                pq = sb("pq", [128, 24, 16], F32, ph)
                tabC = sb("tabC", [128, 16, NC_ + 1], F32, ph)
                tabS = sb("tabS", [128, 16, NC_ + 1], F32, ph)
                CKr = [sb("CKr%d" % i, [128, 16, 128], BF16, ph) for i in range(4)]
                CKi = [sb("CKi%d" % i, [128, 16, 128], BF16, ph) for i in range(4)]
                KT = [sb("KT%d" % i, [128, 4, 128], BF16, ph) for i in range(4)]
                BBr = [sb("BBr%d" % i, [128, 16, 128], BF16, ph) for i in range(4)]
                BBi = [sb("BBi%d" % i, [128, 16, 128], BF16, ph) for i in range(4)]
                dpq = sb("dpq", [128, 4], F32, ph)
                carry = sb("carry", [128, 16, 4], F32, ph)
                Xc = sb("Xc", [128, 2, 16], BF16, ph)
                halfpi = sb("halfpi", [128, 1], F32, ph)
                P.dma("sp", lpq[:], lampq_d[l], "ldp", writes=["lpq"])
                P.dma("sp", dpq[:], dpq_d[l], "ldp", writes=["dpq"])
                P.op("dve", lambda e: e.memset(halfpi[:], math.pi / 2), writes=["halfpi"])
                P.op("dve", lambda e: e.memset(Xc[:], 0.0), writes=["Xc"])
                PQ = lambda i: pq[:, i, :]
                act(PQ(0), lpq[:, :, 2], AF.Exp, ["lpq"], ["pq"])
                tt("dve", PQ(1), lpq[:, :, 0], PQ(0), ALU.mult, ["lpq", "pq"], ["pq"])
                act(PQ(1), PQ(1), AF.Exp, ["pq"], ["pq"])
                tt("dve", PQ(2), lpq[:, :, 1], PQ(0), ALU.mult, ["lpq", "pq"], ["pq"])
                with contextlib.ExitStack() as ph2:
                    NTMP = 516
                    tA = sb("tA", [128, NTMP], F32, ph2)
                    tB = sb("tB", [128, NTMP], F32, ph2)
                    tI = sb("tI", [128, NTMP], I32, ph2)
                    angt = sb("angt", [128, NTMP], F32, ph2)

                    TT_ = dict(tA=tA, tB=tB, tI=tI)

                    def sincos(ang_ap, n, out_c, out_s, res_in, res_c, res_s):
                        a_, b_, i_ = TT_["tA"][:, 0:n], TT_["tB"][:, 0:n], TT_["tI"][:, 0:n]
                        ts("dve", i_, ang_ap, 1.0 / TWO_PI, None, ALU.mult, None, [res_in], ["tI"])
                        cp("dve", a_, i_, ["tI"], ["tA"])
                        stt(a_, a_, -TWO_PI, ang_ap, ALU.mult, ALU.add, ["tA", res_in], ["tA"])
                        act(b_, a_, AF.Sin, ["tA"], ["tB"], scale=0.25, bias=halfpi[:, 0:1])
                        act(a_, a_, AF.Sin, ["tA", "tB"], ["tA"], scale=0.25)
                        tt("dve", b_, b_, a_, ALU.mult, ["tA", "tB"], ["tB"])
                        tt("dve", a_, a_, a_, ALU.mult, ["tA", "tB"], ["tA"])
                        ts("dve", a_, a_, -2.0, 1.0, ALU.mult, ALU.add, ["tA"], ["tA"])
                        stt(out_s, b_, 4.0, a_, ALU.mult, ALU.mult, ["tA", "tB"], [res_s])
                        tt("dve", b_, b_, b_, ALU.mult, ["tB", res_s], ["tB"])
                        ts("dve", out_c, b_, -8.0, 1.0, ALU.mult, ALU.add, ["tB"], [res_c])

                    def cmul(o_r, o_i, a_r, a_i, b_r, b_i, t1_, t2_, res):
                        tt("dve", t1_, a_r, b_r, ALU.mult, res, res)
                        tt("dve", t2_, a_i, b_i, ALU.mult, res, res)
                        tt("dve", o_r, t1_, t2_, ALU.subtract, res, res)
                        tt("dve", t1_, a_r, b_i, ALU.mult, res, res)
                        tt("dve", t2_, a_i, b_r, ALU.mult, res, res)
                        tt("dve", o_i, t1_, t2_, ALU.add, res, res)

                    sincos(PQ(2), 16, PQ(3), PQ(4), "pq", "pq", "pq")
                    tt("dve", PQ(5), PQ(3), PQ(1), ALU.mult, ["pq"], ["pq"])
                    tt("dve", PQ(6), PQ(4), PQ(1), ALU.mult, ["pq"], ["pq"])
                    cmul(PQ(7), PQ(8), PQ(5), PQ(6), PQ(5), PQ(6), PQ(15), PQ(16), ["pq"])
                    cmul(PQ(9), PQ(10), PQ(7), PQ(8), PQ(5), PQ(6), PQ(15), PQ(16), ["pq"])
                    cmul(PQ(11), PQ(12), PQ(9), PQ(10), PQ(5), PQ(6), PQ(15), PQ(16), ["pq"])
                    tt("dve", PQ(20), PQ(1), PQ(1), ALU.mult, ["pq"], ["pq"])
                    tt("dve", PQ(20), PQ(20), PQ(20), ALU.mult, ["pq"], ["pq"])
                    ts("dve", PQ(21), PQ(2), 4.0, None, ALU.mult, None, ["pq"], ["pq"])
                    for hf in range(4):
                        js = slice(hf * 4, hf * 4 + 4)
                        tt("dve", angt[:].rearrange("p (j s) -> p j s", j=4),
                           iota_f[:, 0:NC_ + 1].unsqueeze(1).to_broadcast([128, 4, NC_ + 1]),
                           pq[:, 21, js].unsqueeze(2).to_broadcast([128, 4, NC_ + 1]), ALU.mult, ["iota_f", "pq"], ["angt"])
                        sincos(angt[:], 4 * (NC_ + 1), tabC[:, js, :].rearrange("p j s -> p (j s)"),
                               tabS[:, js, :].rearrange("p j s -> p (j s)"), "angt", "tabC", "tabS")
                    lrq, liq = lpq[:, :, 0], lpq[:, :, 1]
                    ts("dve", PQ(17), PQ(5), -1.0, None, ALU.add, None, ["pq"], ["pq"])
                    tt("dve", PQ(15), lrq, lrq, ALU.mult, ["lpq", "pq"], ["pq"])
                    tt("dve", PQ(16), liq, liq, ALU.mult, ["lpq", "pq"], ["pq"])
                    tt("dve", PQ(15), PQ(15), PQ(16), ALU.add, ["pq"], ["pq"])
                    P.op("dve", lambda e: e.reciprocal(out=PQ(18), in_=PQ(15)), reads=["pq"], writes=["pq"])
                    tt("dve", PQ(15), PQ(17), lrq, ALU.mult, ["pq", "lpq"], ["pq"])
                    tt("dve", PQ(16), PQ(6), liq, ALU.mult, ["pq", "lpq"], ["pq"])
                    tt("dve", PQ(15), PQ(15), PQ(16), ALU.add, ["pq"], ["pq"])
                    tt("dve", PQ(13), PQ(15), PQ(18), ALU.mult, ["pq"], ["pq"])
                    tt("dve", PQ(15), PQ(6), lrq, ALU.mult, ["pq", "lpq"], ["pq"])
                    tt("dve", PQ(16), PQ(17), liq, ALU.mult, ["pq", "lpq"], ["pq"])
                    tt("dve", PQ(15), PQ(15), PQ(16), ALU.subtract, ["pq"], ["pq"])
                    tt("dve", PQ(14), PQ(15), PQ(18), ALU.mult, ["pq"], ["pq"])
                    apow = [(PQ(5), PQ(6)), (PQ(7), PQ(8)), (PQ(9), PQ(10)), (PQ(11), PQ(12))]
                    with contextlib.ExitStack() as ph3:
                        SA = []
                        for k in range(2):
                            SA.append(dict(cre=sb("cre%d" % k, [128, 4, 128], F32, ph3), cim=sb("cim%d" % k, [128, 4, 128], F32, ph3),
                                           k1=sb("k1_%d" % k, [128, 4, 128], F32, ph3), k2=sb("k2_%d" % k, [128, 4, 128], F32, ph3),
                                           bTr=sb("bTr%d" % k, [128, 4, 128], F32, ph3), bTi=sb("bTi%d" % k, [128, 4, 128], F32, ph3),
                                           kr=sb("kr%d" % k, [128, 4, 128], F32, ph3), ki=sb("ki%d" % k, [128, 4, 128], F32, ph3)))
                        C0r = sb("C0r", [128, 16, 128], BF16, ph3)
                        C0i = sb("C0i", [128, 16, 128], BF16, ph3)
                        BTrs = [sb("BTr%d" % i, [128, 16, 128], BF16, ph3) for i in range(2)]
                        BTis = [sb("BTi%d" % i, [128, 16, 128], BF16, ph3) for i in range(2)]
                        bc = lambda ap, js: ap[:, js].unsqueeze(2).to_broadcast([128, 4, 128])

                        def stageA(hf, k):
                            T = SA[k]
                            N = lambda nm: (nm, k)
                            cre, cim, k1, k2 = T["cre"], T["cim"], T["k1"], T["k2"]
                            js = slice(hf * 4, hf * 4 + 4)
                            P.dma("sp", cre[:], cblk_re_d[l][:, js, :], "ld", writes=[N("cre")])
                            P.dma("sp", cim[:], cblk_im_d[l][:, js, :], "ld", writes=[N("cim")])
                            cp("act", C0r[:, js, :], cre[:], [N("cre")], ["C0r"]); yield
                            act(C0i[:, js, :], cim[:], AF.Copy, [N("cim")], ["C0i"], scale=-1.0); yield
                            for jo in range(4):
                                pr, pi = apow[jo]
                                tt("dve", k1[:], cre[:], bc(pr, js), ALU.mult, [N("cre"), "pq"], [N("k1")]); yield
                                tt("dve", k2[:], cim[:], bc(pi, js), ALU.mult, [N("cim"), "pq"], [N("k2")]); yield
                                tt("dve", CKr[jo][:, js, :], k1[:], k2[:], ALU.subtract, [N("k1"), N("k2")], [("CK", jo)]); yield
                                tt("dve", k1[:], cre[:], bc(pi, js), ALU.mult, [N("cre"), "pq", N("k1")], [N("k1")]); yield
                                tt("dve", k2[:], cim[:], bc(pr, js), ALU.mult, [N("cim"), "pq", N("k2")], [N("k2")]); yield
                                stt(CKi[jo][:, js, :], k1[:], -1.0, k2[:], ALU.mult, ALU.subtract, [N("k1"), N("k2")], [("CK", jo)]); yield

                        interleave([stageA(0, 0), stageA(1, 1)])
                        interleave([stageA(2, 0), stageA(3, 1)])

                        def stageB(tau, hf, k, qr_, qi_):
                            T = SA[k]
                            N = lambda nm: (nm, k)
                            bTr, bTi, k1, k2, kr, ki = T["bTr"], T["bTi"], T["k1"], T["k2"], T["kr"], T["ki"]
                            BTr, BTi = BTrs[tau % 2], BTis[tau % 2]
                            js = slice(hf * 4, hf * 4 + 4)
                            P.dma("sp", bTr[:], bblkT_re_d[l][:, js, :], "ld", writes=[N("bTr")])
                            P.dma("sp", bTi[:], bblkT_im_d[l][:, js, :], "ld", writes=[N("bTi")])
                            tt("dve", k1[:], bTr[:], bc(qr_, js), ALU.mult, [N("bTr"), "pq", N("k1")], [N("k1")]); yield
                            tt("dve", k2[:], bTi[:], bc(qi_, js), ALU.mult, [N("bTi"), "pq", N("k2")], [N("k2")]); yield
                            tt("dve", kr[:], k1[:], k2[:], ALU.subtract, [N("k1"), N("k2")], [N("kr")]); yield
                            cp("act", BTr[:, js, :], kr[:], [N("kr")], [("BTr", tau % 2)])
                            br_ = 2 + 2 * k
                            for jq in range(4):
                                P.op("pe", lambda e, jq=jq: e.transpose(ps[br_][:, jq * 128:(jq + 1) * 128], kr[:, jq, :], ident[:]),
                                     reads=[N("kr"), "ident"], writes=[PSR[br_]])
                            cp("act", BBr[tau][:, js, :].rearrange("p j c -> p (j c)"), ps[br_][:], [PSR[br_]], [("BB", tau)]); yield
                            tt("dve", k1[:], bTi[:], bc(qr_, js), ALU.mult, [N("bTi"), "pq", N("k1")], [N("k1")]); yield
                            tt("dve", k2[:], bTr[:], bc(qi_, js), ALU.mult, [N("bTr"), "pq", N("k2")], [N("k2")]); yield
                            tt("dve", ki[:], k1[:], k2[:], ALU.add, [N("k1"), N("k2")], [N("ki")]); yield
                            cp("act", BTi[:, js, :], ki[:], [N("ki")], [("BTi", tau % 2)])
                            bi_ = 3 + 2 * k
                            for jq in range(4):
                                P.op("pe", lambda e, jq=jq: e.transpose(ps[bi_][:, jq * 128:(jq + 1) * 128], ki[:, jq, :], ident[:]),
                                     reads=[N("ki"), "ident"], writes=[PSR[bi_]])
                            cp("act", BBi[tau][:, js, :].rearrange("p j c -> p (j c)"), ps[bi_][:], [PSR[bi_]], [("BB", tau)]); yield

                        for tau in range(4):
                            if tau == 0:
                                qr_, qi_ = PQ(13), PQ(14)
                            else:
                                cmul(pq[:, 22 + 0, :] if False else PQ(22), PQ(23), PQ(13), PQ(14), apow[tau - 1][0], apow[tau - 1][1], PQ(15), PQ(16), ["pq"])
                                qr_, qi_ = PQ(22), PQ(23)
                            interleave([stageB(tau, 0, 0, qr_, qi_), stageB(tau, 1, 1, qr_, qi_)])
                            interleave([stageB(tau, 2, 0, qr_, qi_), stageB(tau, 3, 1, qr_, qi_)])
                            BTr, BTi = BTrs[tau % 2], BTis[tau % 2]
                            for c4 in range(4):
                                for q4 in range(4):
                                    jg = c4 * 4 + q4
                                    mm(ps[tau % 2][:, c4 * 128:(c4 + 1) * 128], BTr[:, jg, :], C0r[:, jg, :], q4 == 0, False,
                                       [("BTr", tau % 2), "C0r"], [PSR[tau % 2]])
                                    mm(ps[tau % 2][:, c4 * 128:(c4 + 1) * 128], BTi[:, jg, :], C0i[:, jg, :], False, q4 == 3,
                                       [("BTi", tau % 2), "C0i"], [PSR[tau % 2]])
                            cp("act", KT[tau][:].rearrange("p c n -> p (c n)"), ps[tau % 2][:], [PSR[tau % 2]], [("KT", tau)])
                        P.flush()
                with contextlib.ExitStack() as ph2:
                    w_u = sb("w_u", [128, 8, 512], BF16, ph2)
                    wglu = sb("wglu", [128, 4, 1024], BF16, ph2)
                    wload(w_u[:], w_in_v[:, :, 0:512], "w0", "w_u")
                    wload(wglu[:], wglu_d[l].rearrange("(c p) d -> p c d", p=128), "w1", "wglu")
                    u_bfs = [sb("u_bf%d" % i, [128, 4, 512], BF16, ph2) for i in range(2)]
                    dus = [sb("du%d" % i, [128, 4, 512], F32, ph2) for i in range(2)]
                    XR = sb("XR", [128, 3, 4, NC_ + 2], BF16, ph2)
                    XI = sb("XI", [128, 3, 4, NC_ + 2], BF16, ph2)
                    YG = sb("YG", [128, 4, 512], BF16, ph2)
                    mk = lambda nm: [sb("%s_%d" % (nm, i), [128, NC_], F32, ph2) for i in range(4)]
                    t1, t2, wr_, wi_, vr_, vi_, p1, p2 = mk("t1"), mk("t2"), mk("wr"), mk("wi"), mk("vr"), mk("vi"), mk("p1"), mk("p2")
                    yy = sb("yy", [128, 512], F32, ph2)
                    y2 = sb("y2", [128, 512], F32, ph2)
                    ssm_pend = []
                    for tb in range(NB):
                        tcols = slice(tb * 512, (tb + 1) * 512)
                        u_bf, du, ub = u_bfs[tb % 2], dus[tb % 2], tb % 2
                        for c4 in range(4):
                            b = c4 % 2
                            for k in range(8):
                                mm(ps[b][:], w_u[:, k, c4 * 128:(c4 + 1) * 128], hT[:, k, tcols], k == 0, k == 7,
                                   ["w_u", ("hT", tb)], [PSR[b]])
                            act(u_bf[:, c4, :], ps[b][:], AF.Copy, [PSR[b]], [("u_bf", ub, c4)])
                            act(du[:, c4, :], ps[b][:], AF.Copy, [PSR[b], "dpq"], [("du", ub, c4)], scale=dpq[:, c4:c4 + 1])
                        u4 = u_bf[:].rearrange("p c (n f) -> p c n f", f=4)
                        def ssm_j(j, tb=tb, u4=u4, tcols=tcols, ub=ub):
                            c4, jj, par = j // 4, j % 4, (tb * 4 + j // 4) % 3
                            b4 = 2 + (j % 4)
                            for i in range(4):
                                mm(ps[b4][:, 0:NC_], BBr[3 - i][:, j, :], u4[:, c4, :, i], i == 0, i == 3,
                                   [("BB", 3 - i), ("u_bf", ub, c4)], [PSR[b4]])
                            for i in range(4):
                                mm(ps[b4][:, NC_:2 * NC_], BBi[3 - i][:, j, :], u4[:, c4, :, i], i == 0, i == 3,
                                   [("BB", 3 - i), ("u_bf", ub, c4)], [PSR[b4]])
                            s_ = j % 4
                            cT, sT = tabC[:, j, 0:NC_], tabS[:, j, 0:NC_]
                            R = lambda nm: (nm, s_)
                            tt("dve", t1[s_][:], cT, ps[b4][:, 0:NC_], ALU.mult, ["tabC", PSR[b4]], [R("t1")])
                            yield
                            tt("dve", t2[s_][:], sT, ps[b4][:, NC_:2 * NC_], ALU.mult, ["tabS", PSR[b4]], [R("t2")])
                            yield
                            tt("dve", wr_[s_][:], t1[s_][:], t2[s_][:], ALU.add, [R("t1"), R("t2")], [R("wr")])
                            yield
                            tt("dve", t1[s_][:], cT, ps[b4][:, NC_:2 * NC_], ALU.mult, ["tabC", PSR[b4], R("t1")], [R("t1")])
                            yield
                            tt("dve", t2[s_][:], sT, ps[b4][:, 0:NC_], ALU.mult, ["tabS", PSR[b4], R("t2")], [R("t2")])
                            yield
                            tt("dve", wi_[s_][:], t1[s_][:], t2[s_][:], ALU.subtract, [R("t1"), R("t2")], [R("wi")])
                            yield
                            if tb > 0:
                                cE, sE = tabC[:, j, NC_:NC_ + 1], tabS[:, j, NC_:NC_ + 1]
                                ts("dve", carry[:, j, 2:3], carry[:, j, 1:2], sE, None, ALU.mult, None,
                                   [("carry", j), "tabS"], [("cinit", j)])
                                stt(carry[:, j, 2:3], carry[:, j, 0:1], cE, carry[:, j, 2:3], ALU.mult, ALU.subtract,
                                    [("carry", j), ("cinit", j), "tabC"], [("cinit", j)])
                                ts("dve", carry[:, j, 3:4], carry[:, j, 0:1], sE, None, ALU.mult, None,
                                   [("carry", j), "tabS"], [("cinit2", j)])
                                stt(carry[:, j, 3:4], carry[:, j, 1:2], cE, carry[:, j, 3:4], ALU.mult, ALU.add,
                                    [("carry", j), ("cinit2", j), "tabC"], [("cinit2", j)])
                                ini_r, ini_i = carry[:, j, 2:3], carry[:, j, 3:4]
                            else:
                                ini_r, ini_i = 0.0, 0.0
                            magb = pq[:, 20, j:j + 1].to_broadcast([128, NC_])
                            P.op("dve", lambda e, s_=s_, magb=magb, ini_r=ini_r: e.tensor_tensor_scan(
                                out=vr_[s_][:], data0=magb, data1=wr_[s_][:], initial=ini_r, op0=ALU.mult, op1=ALU.add),
                                reads=[R("wr"), "pq", ("cinit", j)], writes=[R("vr")])
                            yield
                            P.op("dve", lambda e, s_=s_, magb=magb, ini_i=ini_i: e.tensor_tensor_scan(
                                out=vi_[s_][:], data0=magb, data1=wi_[s_][:], initial=ini_i, op0=ALU.mult, op1=ALU.add),
                                reads=[R("wi"), "pq", ("cinit2", j)], writes=[R("vi")])
                            yield
                            cp("dve", carry[:, j, 0:1], vr_[s_][:, NC_ - 1:NC_], [R("vr")], [("carry", j)])
                            yield
                            cp("dve", carry[:, j, 1:2], vi_[s_][:, NC_ - 1:NC_], [R("vi"), ("carry", j)], [("carry", j)])
                            yield
                            xres = ("X", par, jj)
                            cp("pool", XR[:, par, jj, 0:1], Xc[:, 0, j:j + 1], [("Xc", j)], [xres])
                            yield
                            cp("pool", XI[:, par, jj, 0:1], Xc[:, 1, j:j + 1], [("Xc", j), xres], [xres])
                            yield
                            tt("pool", p1[s_][:], cT, vr_[s_][:], ALU.mult, ["tabC", R("vr")], [R("p1")])
                            yield
                            tt("pool", p2[s_][:], sT, vi_[s_][:], ALU.mult, ["tabS", R("vi")], [R("p2")])
                            yield
                            tt("pool", XR[:, par, jj, 1:NC_ + 1], p1[s_][:], p2[s_][:], ALU.subtract, [R("p1"), R("p2"), xres], [xres])
                            yield
                            tt("pool", p1[s_][:], cT, vi_[s_][:], ALU.mult, ["tabC", R("vi"), R("p1")], [R("p1")])
                            yield
                            tt("pool", p2[s_][:], sT, vr_[s_][:], ALU.mult, ["tabS", R("vr"), R("p2")], [R("p2")])
                            yield
                            tt("pool", XI[:, par, jj, 1:NC_ + 1], p1[s_][:], p2[s_][:], ALU.add, [R("p1"), R("p2"), xres], [xres])
                            yield
                            cp("pool", Xc[:, 0, j:j + 1], XR[:, par, jj, NC_:NC_ + 1], [xres], [("Xc", j)])
                            yield
                            cp("pool", Xc[:, 1, j:j + 1], XI[:, par, jj, NC_:NC_ + 1], [xres, ("Xc", j)], [("Xc", j)])
                            yield
                            yield
                        def ssm_back(c4, tb=tb, u4=u4, tcols=tcols, ub=ub, du=du):
                            par = (tb * 4 + c4) % 3
                            yb = 6 + (c4 % 2)
                            for jo in range(4):
                                reg = ps[yb][:, jo * NC_:(jo + 1) * NC_]
                                for q4 in range(4):
                                    jg = c4 * 4 + q4
                                    mm(reg, CKr[jo][:, jg, :], XR[:, par, q4, 0:NC_], q4 == 0, False, [("CK", jo), ("X", par, q4)], [PSR[yb]])
                                    mm(reg, CKi[jo][:, jg, :], XI[:, par, q4, 0:NC_], False, False, [("CK", jo), ("X", par, q4)], [PSR[yb]])
                                for i in range(jo + 1):
                                    mm(reg, KT[jo - i][:, c4, :], u4[:, c4, :, i], False, i == jo, [("KT", jo - i), ("u_bf", ub, c4)], [PSR[yb]])
                                yield
                            tt("dve", yy[:].rearrange("p (c f) -> p c f", f=4), ps[yb][:].rearrange("p (f c) -> p c f", f=4),
                               du[:, c4, :].rearrange("p (c f) -> p c f", f=4), ALU.add, [PSR[yb], ("du", ub, c4)], ["yy"])
                            yield
                            act(y2[:], yy[:], AF.Square, ["yy"], ["y2"], scale=math.sqrt(0.044715))
                            yield
                            stt(y2[:], y2[:], 1.0, yy[:], ALU.add, ALU.mult, ["y2", "yy"], ["y2"])
                            yield
                            act(y2[:], y2[:], AF.Sigmoid, ["y2"], ["y2"], scale=1.5957691216057308)
                            yield
                            tt("dve", YG[:, c4, :], y2[:], yy[:], ALU.mult, ["y2", "yy"], [("YG", c4)])
                            yield

                            if c4 == 3:
                                for wc in range(4):
                                    bv, bg = (0, 1)
                                    for cg in range(4):
                                        mm(ps[bv][:], wglu[:, cg, wc * 128:(wc + 1) * 128], YG[:, cg, :], cg == 0, cg == 3,
                                           ["wglu", ("YG", cg)], [PSR[bv]])
                                    for cg in range(4):
                                        mm(ps[bg][:], wglu[:, cg, 512 + wc * 128:512 + (wc + 1) * 128], YG[:, cg, :], cg == 0, cg == 3,
                                           ["wglu", ("YG", cg)], [PSR[bg]])
                                    act(y2[:], ps[bg][:], AF.Sigmoid, [PSR[bg]], ["y2"])
                                    yield
                                    tt("dve", yT0[:, wc, tcols], ps[bv][:], y2[:], ALU.mult, [PSR[bv], "y2"], [("yT", wc, tb)])
                                    yield
                        for c4 in range(4):
                            gens = [ssm_j(c4 * 4 + i) for i in range(4)]
                            if len(ssm_pend) >= 2:
                                gens.append(ssm_pend.pop(0))
                            interleave(gens)
                            ssm_pend.append(ssm_back(c4))
                    for g_ in ssm_pend:
                        interleave([g_])
                    P.flush()
            if stop == "ssm":
                P.dma("sp", dbg_d["yT"][:, 0:4, :], yT0[:], "dbg", reads=[])
                P.flush()
                lay.close()
                break
            def attention(qT_ap_fn, kT_ap_fn, V_ap_fn, dvp, bias_fn, scale, sbanks, accbank_fn, pts, qb, rres, tagw):
                nk = 4 * qb + 4
                LAG = 2
                started = set()
                slots = {}
                for step in range(nk + LAG):
                    if step < nk:
                        kt = step
                        r = kt - 4 * qb
                        c0 = max(0, r) * 128
                        sl_ = attention.rr % 3
                        attention.rr += 1
                        sbk = sbanks[sl_]
                        pt = pts[sl_]
                        ptres = ("pt", sl_)
                        slots[kt] = (pt, ptres)
                        mm(ps[sbk][:, c0:512], kT_ap_fn(kt), qT_ap_fn(slice(qb * 512 + c0, qb * 512 + 512)), True, True,
                           rres, [PSR[sbk]])
                        kw = dict(scale=scale)
                        if bias_fn is not None:
                            kw["bias"] = bias_fn(kt)
                        act(pt[:, c0:512], ps[sbk][:, c0:512], AF.Exp, [PSR[sbk]] + rres, [ptres], **kw)
                        if r >= 0:
                            tt("dve", pt[:, c0:c0 + 128], pt[:, c0:c0 + 128], tri[:], ALU.mult, [ptres, "tri"], [ptres])
                    kt = step - LAG
                    if kt >= 0:
                        r = kt - 4 * qb
                        pt, ptres = slots[kt]
                        for qs in range(max(0, r), 4):
                            bk, ap = accbank_fn(qs)
                            first = bk not in started
                            started.add(bk)
                            mm(ap, pt[:, qs * 128:(qs + 1) * 128], V_ap_fn(kt), first, kt == 4 * qb + qs,
                               [ptres] + rres, [PSR[bk]])
                    yield

            pend = [None]

            def run_unit(att_gen, epi_gen):
                gens = [att_gen] + ([pend[0]] if pend[0] is not None else [])
                interleave(gens)
                pend[0] = epi_gen

            def drain_pending():
                if pend[0] is not None:
                    interleave([pend[0]])
                    pend[0] = None

            def rr2(gs):
                gs = list(gs)
                while gs:
                    for g in list(gs):
                        try:
                            next(g)
                        except StopIteration:
                            gs.remove(g)
                        yield
            attention.rr = 0

            yT1 = sb("yT1", [128, 4, S], BF16, lay)
            with contextlib.ExitStack() as ph:
                wq = sb("wq", [128, 8, 512], BF16, ph)
                wk = sb("wk", [128, 8, 512], BF16, ph)
                wv = sb("wv", [128, 8, 512], BF16, ph)
                wqr = sb("wqr", [128, 8, 512], BF16, ph)
                wkr = sb("wkr", [128, 8, 512], BF16, ph)
                ropec = sb("ropec", [128, S], F32, ph)
                ropes = sb("ropes", [128, S], F32, ph)
                qT = sb("qT", [128, S], BF16, ph)
                kT = sb("kT", [128, S], BF16, ph)
                kTz = [sb("kTz%d" % i, [128, S], BF16, ph) for i in range(2)]
                P.op("pool", lambda e: e.memset(kTz[0][64:128, :], 0.0), writes=["kTz0"])
                P.op("pool", lambda e: e.memset(kTz[1][0:64, :], 0.0), writes=["kTz1"])
                Vp = sb("Vp", [128, NT, 4, 129], BF16, ph)
                pts = [sb("pt%d" % i, [128, 512], BF16, ph) for i in range(3)]
                ldr = sb("ldr", [128, 4, 64], F32, ph)
                lsc = sb("lsc", [128, 8], F32, ph)
                gsc = sb("gsc", [128, 512], F32, ph)
                rt1 = sb("rt1", [128, 512], F32, ph)
                rt2 = sb("rt2", [128, 512], F32, ph)
                rt3 = sb("rt3", [128, 512], F32, ph)
                rt4 = sb("rt4", [128, 512], F32, ph)
                o1 = sb("o1", [128, 4, 128], F32, ph)
                ods = [sb("od%d" % i, [128, 128], F32, ph) for i in range(2)]
                onfs = [sb("onf%d" % i, [128, 128], F32, ph) for i in range(2)]
                junks = [sb("junk%d" % i, [128, 128], F32, ph) for i in range(2)]
                sms = [sb("sm%d" % i, [128, 8], F32, ph) for i in range(2)]
                epsr = sb("epsr", [128, 1], F32, ph)
                P.op("dve", lambda e: e.memset(epsr[:], RMS_EPS), writes=["epsr"])
                P.dma("sp", ropec[:], ropec_d, "ldc", writes=["ropec"])
                P.dma("sp", ropes[:], ropes_d, "ldc", writes=["ropes"])
                P.dma("sp", ldr[:], dlam_d[l], "ldp", writes=["ldr"])
                P.dma("sp", gsc[:], dg_d[l], "ldp", writes=["gsc"])
                wload(wq[:], w_in_v[:, :, C_SSM:C_SSM + 512], "w0", "wq")
                wload(wk[:], w_in_v[:, :, C_DQ:C_DQ + 512], "w1", "wk")
                wload(wv[:], w_in_v[:, :, C_DK:C_DK + 512], "w2", "wv")
                ts("dve", gsc[:], gsc[:], 1.0 - lam_init, None, ALU.mult, None, ["gsc"], ["gsc"])
                tt("dve", ldr[:, 0, :], ldr[:, 0, :], ldr[:, 1, :], ALU.mult, ["ldr"], ["ldr"])
                tt("dve", ldr[:, 2, :], ldr[:, 2, :], ldr[:, 3, :], ALU.mult, ["ldr"], ["ldr"])
                P.op("dve", lambda e: e.reduce_sum(out=lsc[:, 0:1], in_=ldr[:, 0, :], axis=AX.X), reads=["ldr"], writes=["lsc"])
                P.op("dve", lambda e: e.reduce_sum(out=lsc[:, 1:2], in_=ldr[:, 2, :], axis=AX.X), reads=["ldr", "lsc"], writes=["lsc"])
                act(lsc[:, 2:4], lsc[:, 0:2], AF.Exp, ["lsc"], ["lsc"])
                tt("dve", lsc[:, 4:5], lsc[:, 3:4], lsc[:, 2:3], ALU.subtract, ["lsc"], ["lsc"])
                ts("dve", lsc[:, 5:6], lsc[:, 4:5], -lam_init, None, ALU.add, None, ["lsc"], ["lsc"])
                neglam = lsc[:, 5:6]
                for (w_, wr__, nm) in ((wq, wqr, "wq"), (wk, wkr, "wk")):
                    v_in = w_[:].rearrange("p k (m two f) -> p (k m) two f", two=2, f=32)
                    v_out = wr__[:].rearrange("p k (m two f) -> p (k m) two f", two=2, f=32)
                    ts("dve", v_out[:, :, 0, :], v_in[:, :, 1, :], -1.0, None, ALU.mult, None, [nm], [nm + "r"])
                    cp("dve", v_out[:, :, 1, :], v_in[:, :, 0, :], [nm, nm + "r"], [nm + "r"])
                P.op("dve", lambda e: e.memset(Vp[:, :, :, 128:129], 1.0), writes=["Vp1"])
                for n in range(NT):
                    vb = 4 + (n % 4)
                    for k in range(8):
                        mm(ps[vb][:], hT[:, k, n * 128:(n + 1) * 128], wv[:, k, :], k == 0, k == 7, ["wv", ("hT", n // 4)], [PSR[vb]])
                    cp("act", Vp[:, n, :, 0:128], ps[vb][:].rearrange("p (h c) -> p h c", h=4), [PSR[vb]], ["Vp"])
                for h in range(4):
                    hc = slice(h * 128, (h + 1) * 128)
                    for tb in range(NB):
                        tcols = slice(tb * 512, (tb + 1) * 512)
                        for (w_, wr__, dst, nm, pa, pb) in ((wq, wqr, qT, "qT", 2, 3), (wk, wkr, kT, "kT", 0, 1)):
                            for k in range(8):
                                mm(ps[pa][:], w_[:, k, hc], hT[:, k, tcols], k == 0, k == 7, [nm[0:1] == "q" and "wq" or "wk", ("hT", tb)], [PSR[pa]])
                            for k in range(8):
                                mm(ps[pb][:], wr__[:, k, hc], hT[:, k, tcols], k == 0, k == 7, [(nm[0:1] == "q" and "wq" or "wk") + "r", ("hT", tb)], [PSR[pb]])
                            rta, rtb = (rt1, rt2) if nm == "qT" else (rt3, rt4)
                            tt("dve", rta[:], ropec[:, tcols], ps[pa][:], ALU.mult, ["ropec", PSR[pa]], [nm + "rt1"])
                            tt("dve", rtb[:], ropes[:, tcols], ps[pb][:], ALU.mult, ["ropes", PSR[pb]], [nm + "rt2"])
                            if nm == "qT":
                                tt("dve", dst[:, tcols], rta[:], rtb[:], ALU.add, [nm + "rt1", nm + "rt2"], [nm])
                            else:
                                tt("dve", kTz[0][0:64, tcols], rta[0:64, :], rtb[0:64, :], ALU.add, [nm + "rt1", nm + "rt2"], [nm, "kTz0"])
                                tt("dve", kTz[1][64:128, tcols], rta[64:128, :], rtb[64:128, :], ALU.add, [nm + "rt1", nm + "rt2"], [nm, "kTz1"])
                    def diff_epi_qs(h, hc, qb, m, ab, qs):
                        k = qs % 2
                        sm_, od_, onf_, junk_ = sms[k], ods[k], onfs[k], junks[k]
                        R = lambda nm: (nm, k)
                        bk = ab + qs // 2
                        acc = ps[bk][:, (qs % 2) * 129:(qs % 2) * 129 + 129]
                        P.op("dve", lambda e: e.reciprocal(out=sm_[:, 0:1], in_=acc[:, 128:129]), reads=[PSR[bk]], writes=[R("sm0")])
                        yield
                        if m == 0:
                            ts("dve", o1[:, qs, :], acc[:, 0:128], sm_[:, 0:1], None, ALU.mult, None, [PSR[bk], R("sm0")], [("o1", qs)])
                            yield
                        else:
                            tt("dve", sm_[:, 1:2], sm_[:, 0:1], neglam, ALU.mult, [R("sm0"), "lsc"], [R("sm1")])
                            yield
                            stt(od_[:], acc[:, 0:128], sm_[:, 1:2], o1[:, qs, :], ALU.mult, ALU.add, [PSR[bk], R("sm1"), ("o1", qs)], [R("od")])
                            yield
                            act(junk_[:], od_[:], AF.Square, [R("od")], [R("junk"), R("sm2")], accum_out=sm_[:, 2:3])
                            yield
                            act(sm_[:, 3:4], sm_[:, 2:3], AF.Ln, [R("sm2")], [R("sm3")], scale=1.0 / 128, bias=epsr[:, 0:1])
                            yield
                            act(sm_[:, 4:5], sm_[:, 3:4], AF.Exp, [R("sm3")], [R("sm4")], scale=-0.5)
                            yield
                            stt(onf_[:], od_[:], sm_[:, 4:5], gsc[:, hc], ALU.mult, ALU.mult, [R("od"), R("sm4"), "gsc"], [R("onf")])
                            yield
                            P.op("pe", lambda e: e.transpose(ps[5][:, qs * 128:(qs + 1) * 128], onf_[:], ident[:]),
                                 reads=[R("onf"), "ident"], writes=[PSR[5]])
                            yield

                    def diff_epi(h, hc, qb, m, ab):
                        yield from rr2([diff_epi_qs(h, hc, qb, m, ab, 0), diff_epi_qs(h, hc, qb, m, ab, 1)])
                        yield from rr2([diff_epi_qs(h, hc, qb, m, ab, 2), diff_epi_qs(h, hc, qb, m, ab, 3)])
                        if m == 1:
                            cp("act", yT1[:, h, qb * 512:(qb + 1) * 512], ps[5][:], [PSR[5]], [("yT", 4 + h, qb)])
                            yield

                    for qb in range(NB):
                        for m in range(2):
                            mr = slice(m * 64, (m + 1) * 64)
                            ab = 3 if m == 0 else 6
                            att = attention(lambda cols: qT[:, cols], lambda kt, m=m: kTz[m][:, kt * 128:(kt + 1) * 128],
                                            lambda kt, h=h: Vp[:, kt, h, :], 129, None, 0.125, [0, 1, 2],
                                            lambda qs, ab=ab: (ab + qs // 2, ps[ab + qs // 2][:, (qs % 2) * 129:(qs % 2) * 129 + 129]),
                                            pts, qb, ["qT", "kT", "kTz0", "kTz1", "Vp", "Vp1"], "d")
                            run_unit(att, diff_epi(h, hc, qb, m, ab))
                drain_pending()
                P.flush()
            if stop == "diff":
                P.dma("sp", dbg_d["yT"][:, 0:4, :], yT0[:], "dbg", reads=[])
                P.dma("sp", dbg_d["yT"][:, 4:8, :], yT1[:], "dbg", reads=[])
                P.flush()
                lay.close()
                break

            yT2 = sb("yT2", [128, 4, S], BF16, lay)
            with contextlib.ExitStack() as ph:
                wq = sb("fwq", [128, 8, 512], BF16, ph)
                wk = sb("fwk", [128, 8, 512], BF16, ph)
                wv = sb("fwv", [128, 8, 512], BF16, ph)
                wf = sb("fwf", [128, 8, 8], BF16, ph)
                QA = [sb("QA%d" % i, [128, S], BF16, ph) for i in range(2)]
                KA = [sb("KA%d" % i, [128, S], BF16, ph) for i in range(2)]
                Vp = sb("fVp", [128, NT, 8, 65], BF16, ph)
                pts = [sb("fpt%d" % i, [128, 512], BF16, ph) for i in range(3)]
                fbt = sb("fbt", [8, 2], F32, ph)
                zz = sb("zz", [8, S], F32, ph)
                z2 = sb("z2", [8, S], F32, ph)
                z3 = sb("z3", [8, S], F32, ph)
                chm = sb("chm", [8, 3, S], BF16, ph)
                chn = sb("chn", [8, 3, S], BF16, ph)
                of_ = sb("of_", [128, 4, 128], F32, ph)
                sms = [sb("fsm%d" % i, [128, 8], F32, ph) for i in range(2)]
                P.dma("sp", fbt[:, 0:1], fb_d[l], "ldp", writes=["fbt"])
                wload(wq[:], w_in_v[:, :, C_DV:C_DV + 512], "w0", "fwq")
                wload(wk[:], w_in_v[:, :, C_FQ:C_FQ + 512], "w1", "fwk")
                wload(wv[:], w_in_v[:, :, C_FK:C_FK + 512], "w2", "fwv")
                wload(wf[:], w_in_v[:, :, C_FV:C_FV + 8], "w3", "fwf")
                ts("dve", fbt[:, 1:2], fbt[:, 0:1], -1.0, None, ALU.mult, None, ["fbt"], ["fbt1"])
                for tb in range(NB):
                    tcols = slice(tb * 512, (tb + 1) * 512)
                    for k in range(8):
                        mm(ps[6][0:8, :], wf[:, k, :], hT[:, k, tcols], k == 0, k == 7, ["fwf", ("hT", tb)], [PSR[6]])
                    act(zz[:, tcols], ps[6][0:8, :], AF.Identity, [PSR[6], "fbt1"], ["zz"], scale=-1.0, bias=fbt[:, 1:2])
                ts("dve", z2[:], zz[:], -1.0, None, ALU.mult, None, ["zz"], ["z2"])
                tt("dve", z2[:], z2[:], zz[:], ALU.max, ["z2", "zz"], ["z2"])
                act(z2[:], z2[:], AF.Exp, ["z2"], ["z2"], scale=-1.0)
                act(z2[:], z2[:], AF.Ln, ["z2"], ["z2"], bias=1.0)
                ts("dve", zz[:], zz[:], 0.0, None, ALU.max, None, ["zz"], ["zz"])
                tt("dve", zz[:], zz[:], z2[:], ALU.add, ["zz", "z2"], ["zz"])
                P.op("dve", lambda e: e.memset(z2[:], 1.0), reads=["z2"], writes=["z2"])
                P.op("dve", lambda e: e.tensor_tensor_scan(out=z3[:], data0=z2[:], data1=zz[:], initial=0.0,
                                                           op0=ALU.mult, op1=ALU.add), reads=["z2", "zz"], writes=["z3"])
                ts("dve", chm[:, 0, :], z3[:], -1.0, None, ALU.mult, None, ["z3"], ["chm0"])
                stt(zz[:], z3[:], -1.0, chm[:, 0, :], ALU.mult, ALU.subtract, ["z3", "chm0", "zz"], ["zz"])
                cp("dve", chm[:, 1, :], zz[:], ["zz"], ["chm1"])
                tt("dve", z2[:], zz[:], chm[:, 1, :], ALU.subtract, ["zz", "chm1", "z2"], ["z2"])
                cp("dve", chm[:, 2, :], z2[:], ["z2"], ["chm2"])
                ts("dve", chn[:].rearrange("h r s -> h (r s)"), chm[:].rearrange("h r s -> h (r s)"), -1.0, None, ALU.mult, None,
                   ["chm0", "chm1", "chm2"], ["chn"])
                P.op("dve", lambda e: e.memset(Vp[:, :, :, 64:65], 1.0), writes=["fVp1"])
                for n in range(NT):
                    vb = 2 + (n % 4)
                    for k in range(8):
                        mm(ps[vb][:], hT[:, k, n * 128:(n + 1) * 128], wv[:, k, :], k == 0, k == 7, ["fwv", ("hT", n // 4)], [PSR[vb]])
                    cp("act", Vp[:, n, :, 0:64], ps[vb][:].rearrange("p (h c) -> p h c", h=8), [PSR[vb]], ["fVp"])
                for i in range(2):
                    P.op("dve", lambda e, i=i: e.memset(KA[i][64:128, :], 0.0), writes=[("KA1", i)])
                    P.op("dve", lambda e, i=i: e.memset(QA[i][64:128, :], 0.0), writes=[("QAc", i)])
                    P.op("dve", lambda e, i=i: e.memset(KA[i][64:67, :], 1.0), writes=[("KA1", i)])
                    P.op("dve", lambda e, i=i: e.memset(QA[i][96:99, :], 1.0), writes=[("QAc", i)])
                for hp in range(4):
                    hc = slice(hp * 128, (hp + 1) * 128)
                    for tb in range(NB):
                        tcols = slice(tb * 512, (tb + 1) * 512)
                        for k in range(8):
                            mm(ps[0][:], wq[:, k, hc], hT[:, k, tcols], k == 0, k == 7, ["fwq", ("hT", tb)], [PSR[0]])
                        act(QA[0][0:64, tcols], ps[0][0:64, :], AF.Copy, [PSR[0]], [("QA", 0)], scale=0.125)
                        act(QA[1][0:64, tcols], ps[0][64:128, :], AF.Copy, [PSR[0]], [("QA", 1)], scale=0.125)
                        for k in range(8):
                            mm(ps[1][:], wk[:, k, hc], hT[:, k, tcols], k == 0, k == 7, ["fwk", ("hT", tb)], [PSR[1]])
                        cp("dve", KA[0][0:64, tcols], ps[1][0:64, :], [PSR[1]], [("KA", 0)])
                        cp("dve", KA[1][0:64, tcols], ps[1][64:128, :], [PSR[1]], [("KA", 1)])
                    for i in range(2):
                        h = hp * 2 + i
                        for r3 in range(3):
                            P.dma("sp", QA[i][64 + r3:65 + r3, :], chm[h:h + 1, r3, :], "ldf%d" % i,
                                  reads=["chm0", "chm1", "chm2"], writes=[("QAc", i)])
                            P.dma("sp", KA[i][96 + r3:97 + r3, :], chn[h:h + 1, r3, :], "ldf%d" % i,
                                  reads=["chn"], writes=[("KA1", i)])
                    def fox_epi(hp, qb, i):
                        fb_ = (3, 4, 6, 7)[i + 2 * (qb % 2)]
                        for qs in range(4):
                            k = qs % 2
                            acc = ps[fb_][:, qs * 65:qs * 65 + 65]
                            P.op("dve", lambda e, acc=acc, k=k: e.reciprocal(out=sms[k][:, 0:1], in_=acc[:, 64:65]),
                                 reads=[PSR[fb_]], writes=[("fsm0", k)])
                            yield
                            ts("dve", of_[:, qs, i * 64:(i + 1) * 64], acc[:, 0:64], sms[k][:, 0:1], None, ALU.mult, None,
                               [PSR[fb_], ("fsm0", k)], [("of", qs)])
                            yield
                        if i == 1:
                            for qs in range(4):
                                P.op("pe", lambda e, qs=qs: e.transpose(ps[5][:, qs * 128:(qs + 1) * 128], of_[:, qs, :], ident[:]),
                                     reads=[("of", qs), "ident"], writes=[PSR[5]])
                                yield
                            cp("act", yT2[:, hp, qb * 512:(qb + 1) * 512], ps[5][:], [PSR[5]], [("yT", 8 + hp, qb)])
                            yield

                    for qb in range(NB):
                        for i in range(2):
                            h = hp * 2 + i
                            att = attention(lambda cols, i=i: QA[i][:, cols], lambda kt, i=i: KA[i][:, kt * 128:(kt + 1) * 128],
                                            lambda kt, h=h: Vp[:, kt, h, :], 65, None, 1.0, [0, 1, 2],
                                            lambda qs, i=i, qb=qb: ((3, 4, 6, 7)[i + 2 * (qb % 2)], ps[(3, 4, 6, 7)[i + 2 * (qb % 2)]][:, qs * 65:qs * 65 + 65]),
                                            pts, qb, [("QA", i), ("QAc", i), ("KA", i), ("KA1", i), "fVp", "fVp1"], "f")
                            run_unit(att, fox_epi(hp, qb, i))
                drain_pending()
                P.flush()
            if stop == "fox":
                P.dma("sp", dbg_d["yT"][:, 0:4, :], yT0[:], "dbg", reads=[])
                P.dma("sp", dbg_d["yT"][:, 4:8, :], yT1[:], "dbg", reads=[])
                P.dma("sp", dbg_d["yT"][:, 8:12, :], yT2[:], "dbg", reads=[])
                P.flush()
                lay.close()
                break
            mg = contextlib.ExitStack()
            mergedT = sb("mergedT", [128, 8, S], BF16, mg)
            with contextlib.ExitStack() as ph:
                wg2 = [sb("wg2_%d" % i, [128, 8, 3, 128], BF16, ph) for i in range(2)]
                wb2 = [sb("wb2_%d" % i, [128, 12, 128], BF16, ph) for i in range(2)]
                sgs = [sb("sgs%d" % i, [128, 512], F32, ph) for i in range(3)]
                mt = [sb("mt%d" % i, [128, 512], F32, ph) for i in range(2)]
                wbv = w_br_d[l].rearrange("(c p) d -> p c d", p=128)
                for dc in range(8):
                    s_ = dc % 2
                    for n3 in range(3):
                        c0 = C_FF + n3 * 1024 + dc * 128
                        P.dma("pool", wg2[s_][:, :, n3, :], w_in_v[:, :, c0:c0 + 128], "wg%d" % s_, writes=[("wg2", s_)])
                    P.dma("pool", wb2[s_][:], wbv[:, :, dc * 128:(dc + 1) * 128], "wb%d" % s_, writes=[("wb2", s_)])
                    for tb in range(NB):
                        tcols = slice(tb * 512, (tb + 1) * 512)
                        for n3 in range(3):
                            for k in range(8):
                                mm(ps[n3][:], wg2[s_][:, k, n3, :], hT[:, k, tcols], k == 0, k == 7, [("wg2", s_), ("hT", tb)], [PSR[n3]])
                            for c in range(4):
                                mm(ps[3 + n3][:], wb2[s_][:, n3 * 4 + c, :], (yT0, yT1, yT2)[n3][:, c, tcols], c == 0, c == 3,
                                   [("wb2", s_), ("yT", n3 * 4 + c, tb)], [PSR[3 + n3]])
                            act(sgs[n3][:], ps[n3][:], AF.Sigmoid, [PSR[n3]], [("sgs", n3)])
                        tt("dve", mt[0][:], sgs[0][:], ps[3][:], ALU.mult, [("sgs", 0), PSR[3]], ["mt0"])
                        tt("dve", mt[1][:], sgs[1][:], ps[4][:], ALU.mult, [("sgs", 1), PSR[4]], ["mt1"])
                        tt("dve", mt[0][:], mt[0][:], mt[1][:], ALU.add, ["mt0", "mt1"], ["mt0"])
                        tt("dve", mt[1][:], sgs[2][:], ps[5][:], ALU.mult, [("sgs", 2), PSR[5], "mt1"], ["mt1"])
                        tt("dve", mergedT[:, dc, tcols], mt[0][:], mt[1][:], ALU.add, ["mt0", "mt1"], [("mergedT", tb)])
                P.flush()
            x_src = x_d if l == 0 else x_scr
            with contextlib.ExitStack() as ph:
                wo = sb("wo", [128, 8, D], BF16, ph)
                lng = sb("lng", [128, D], F32, ph)
                lnb = sb("lnb", [128, D], F32, ph)
                xin = [sb("xl%d" % i, [128, D], F32, ph) for i in range(4)]
                zb = [sb("zb%d" % i, [128, D], F32, ph) for i in range(4)]
                xo = [sb("xo%d" % i, [128, D], F32, ph) for i in range(4)]
                stp = dict(st=[sb("lnst%d" % i, [128, 2, 6], F32, ph) for i in range(4)], mv=[sb("lnmv%d" % i, [128, 8], F32, ph) for i in range(4)],
                           xn=[sb("lnxn%d" % i, [128, D], F32, ph) for i in range(4)], eps=sb("lneps", [128, 1], F32, ph))
                P.op("dve", lambda e: e.memset(stp["eps"][:], LN_EPS), writes=["lneps"])
                wov = w_out_d[l].rearrange("(k p) c -> p k c", p=128)
                wload(wo[:, :, 0:512], wov[:, :, 0:512], "w0", "wo")
                wload(wo[:, :, 512:1024], wov[:, :, 512:1024], "w1", "wo")
                P.dma("sp", lng[:], lnrep_d[l, 0], "ldp", writes=["lng"])
                P.dma("sp", lnb[:], lnrep_d[l, 1], "ldp", writes=["lnb"])
                def ln1_tile(n):
                    s_ = n % 4
                    P.dma("sp", xin[s_][:], x_src[n * 128:(n + 1) * 128, :], "lx%d" % s_, writes=[("xin", s_)])
                    for hf in range(2):
                        b = 4 + hf + 2 * (s_ % 2)
                        for dc in range(8):
                            mm(ps[b][:], mergedT[:, dc, n * 128:(n + 1) * 128], wo[:, dc, hf * 512:(hf + 1) * 512], dc == 0, dc == 7,
                               ["wo", ("mergedT", n // 4)], [PSR[b]])
                        stt(zb[s_][:, hf * 512:(hf + 1) * 512], xin[s_][:, hf * 512:(hf + 1) * 512], ALPHA, ps[b][:], ALU.mult, ALU.add,
                            [("xin", s_), PSR[b]], [("zb", s_)])
                        yield
                    yield from layer_norm(xo[s_][:], ("xo", s_), zb[s_][:], ("zb", s_), lng[:], lnb[:], stp, s_)
                    P.dma("sp", x_scr[n * 128:(n + 1) * 128, :], xo[s_][:], "sx%d" % s_, reads=[("xo", s_)])
                    transposes(n, [0, 1, 2, 3], xo[s_], ("xo", s_))
                    yield
                for n in range(0, NT, 4):
                    interleave([ln1_tile(n + i) for i in range(4)])
                P.flush()
            mg.close()
            lay.close()
            if stop == "ln1":
                with contextlib.ExitStack() as ph:
                    xt = sb("xt", [128, NT, D], F32, ph)
                    P.dma("sp", xt[:], x_scr.rearrange("(n p) d -> p n d", p=128), "dbg", writes=["xt"])
                    P.dma("sp", dbg_d["x1"], xt[:], "dbg", reads=["xt"])
                    P.flush()
                break
            with contextlib.ExitStack() as ph:
                x_sb = sb("x_sb", [128, NT, D], F32, ph)
                hid = sb("hid", [128, 8, S], BF16, ph)
                wgu = [sb("wgu%d" % i, [128, 8, 512], BF16, ph) for i in range(2)]
                wdn = [sb("wdn%d" % i, [128, 2, D], BF16, ph) for i in range(4)]
                wr = sb("wr", [128, 8, 20], BF16, ph)
                brp = sb("brp", [128, 20], F32, ph)
                sel = sb("sel", [128, 16, 128], BF16, ph)
                gTb = sb("gTb", [128, S], BF16, ph)
                lg = sb("lg", [128, NT, 20], F32, ph)
                rg = sb("rg", [128, 12, NT], F32, ph)
                r4 = [sb("r4_%d" % i, [128, NT, 4], F32, ph) for i in range(6)]
                r16 = sb("r16", [128, NT, 4, 4], F32, ph)
                gate = sb("gate", [128, NT, 16], F32, ph)
                gateT = sb("gateT", [32, S], F32, ph)
                gbs = [sb("gbs%d" % i, [128, 512], F32, ph) for i in range(2)]
                slbuf = sb("slbuf", [128, 4, 512], F32, ph)
                sl = [[slbuf[:, i * 2 + p_, :] for p_ in range(2)] for i in range(2)]
                lng = sb("lng2", [128, D], F32, ph)
                lnb = sb("lnb2", [128, D], F32, ph)
                class _V:
                    def __init__(self, ap):
                        self.ap = ap

                    def __getitem__(self, key):
                        return self.ap
                hidf = hid[:].rearrange("p a s -> p (a s)").bitcast(F32)
                stp = dict(st=[sb("lnst2%d" % i, [128, 2, 6], F32, ph) for i in range(4)], mv=[sb("lnmv2%d" % i, [128, 8], F32, ph) for i in range(4)],
                           xn=[_V(slbuf[:, 2 * k:2 * k + 2, :].rearrange("p a c -> p (a c)")) for k in range(2)] +
                              [_V(hidf[:, k * 1024:(k + 1) * 1024]) for k in range(2)],
                           eps=sb("lneps2", [128, 1], F32, ph))
                P.op("dve", lambda e: e.memset(stp["eps"][:], LN_EPS), writes=["lneps"])
                P.dma("sp", x_sb[:], x_scr.rearrange("(n p) d -> p n d", p=128), "ldx", writes=[("x", n) for n in range(NT)])
                P.dma("sp", lng[:], lnrep_d[l, 2], "ldp", writes=["lng"])
                P.dma("sp", lnb[:], lnrep_d[l, 3], "ldp", writes=["lnb"])
                P.dma("sp", brp[:], brep_d[l], "ldp", writes=["brp"])
                wload(sel[:], sel_d, "w1", "sel")
                P.op("dve", lambda e: e.memset(gTb[:], 0.0), writes=["gTb"])
                wload(wr[:], wr_d[l].rearrange("(k p) c -> p k c", p=128), "w0", "wr")
                lgT = gateT
                for tb in range(NB):
                    tcols = slice(tb * 512, (tb + 1) * 512)
                    for k in range(8):
                        mm(ps[6][0:20, :], wr[:, k, :], hT[:, k, tcols], k == 0, k == 7, ["wr", ("hT", tb)], [PSR[6]])
                    cp("act", lgT[0:20, tcols], ps[6][0:20, :], [PSR[6]], ["gateT"])
                for n in range(NT):
                    b = 6 + n // 8
                    P.op("pe", lambda e, n=n, b=b: e.matmul(ps[b][:, (n % 8) * 64:(n % 8) * 64 + 64], lhsT=lgT[0:20, n * 128:(n + 1) * 128],
                                                            rhs=ident[0:20, 0:64], start=True, stop=True, skip_group_check=True),
                         reads=["gateT", "ident"], writes=[PSR[b]])
                for hb in range(2):
                    tt("dve", lg[:, hb * 8:(hb + 1) * 8, :], ps[6 + hb][:].rearrange("p (n c) -> p n c", n=8)[:, :, 0:20],
                       brp[:].unsqueeze(1).to_broadcast([128, 8, 20]), ALU.add, [PSR[6 + hb], "brp"], ["lg"])
                LG = lg[:, :, 0:4]
                LE = lg[:, :, 4:20].rearrange("p n (g e) -> p n g e", g=4)
                bc4 = lambda ap: ap.unsqueeze(2).to_broadcast([128, NT, 4])
                RG = lambda i: rg[:, i, :]
                P.op("dve", lambda e: e.reduce_max(out=RG(0), in_=LG, axis=AX.X), reads=["lg"], writes=["rg0"])
                tt("dve", r4[0][:], LG, bc4(RG(0)), ALU.subtract, ["lg", "rg0"], ["r4_0"])
                act(r4[0][:], r4[0][:], AF.Exp, ["r4_0"], ["r4_0"])
                P.op("dve", lambda e: e.reduce_sum(out=RG(1), in_=r4[0][:], axis=AX.X), reads=["r4_0"], writes=["rg1"])
                P.op("dve", lambda e: e.reciprocal(out=RG(1), in_=RG(1)), reads=["rg1"], writes=["rg1"])
                tt("dve", r4[1][:], LG, bc4(RG(0)), ALU.is_equal, ["lg", "rg0"], ["r4_1"])
                tt("dve", r16[:], LE, r4[1][:].unsqueeze(3).to_broadcast([128, NT, 4, 4]), ALU.mult, ["lg", "r4_1"], ["r16"])
                P.op("dve", lambda e: e.tensor_reduce(out=r4[2][:], in_=r16[:].rearrange("p n g e -> p n e g"), axis=AX.X, op=ALU.add),
                     reads=["r16"], writes=["r4_2"])
                P.op("dve", lambda e: e.reduce_max(out=RG(2), in_=r4[2][:], axis=AX.X), reads=["r4_2"], writes=["rg2"])
                tt("dve", r4[3][:], r4[2][:], bc4(RG(2)), ALU.is_equal, ["r4_2", "rg2"], ["r4_3"])
                stt(r4[4][:], r4[3][:], -1e30, r4[2][:], ALU.mult, ALU.add, ["r4_3", "r4_2"], ["r4_4"])
                P.op("dve", lambda e: e.reduce_max(out=RG(3), in_=r4[4][:], axis=AX.X), reads=["r4_4"], writes=["rg3"])
                tt("dve", r4[5][:], r4[4][:], bc4(RG(3)), ALU.is_equal, ["r4_4", "rg3"], ["r4_5"])
                tt("dve", RG(4), RG(3), RG(2), ALU.subtract, ["rg3", "rg2"], ["rg4"])
                act(RG(4), RG(4), AF.Exp, ["rg4"], ["rg4"])
                ts("dve", RG(5), RG(4), 1.0, None, ALU.add, None, ["rg4"], ["rg5"])
                P.op("dve", lambda e: e.reciprocal(out=RG(5), in_=RG(5)), reads=["rg5"], writes=["rg5"])
                tt("dve", RG(6), RG(4), RG(5), ALU.mult, ["rg4", "rg5"], ["rg6"])
                tt("dve", RG(5), RG(5), RG(1), ALU.mult, ["rg5", "rg1"], ["rg5"])
                tt("dve", RG(6), RG(6), RG(1), ALU.mult, ["rg6", "rg1"], ["rg6"])
                tt("dve", r4[3][:], r4[3][:], bc4(RG(5)), ALU.mult, ["r4_3", "rg5"], ["r4_3"])
                tt("dve", r4[5][:], r4[5][:], bc4(RG(6)), ALU.mult, ["r4_5", "rg6"], ["r4_5"])
                tt("dve", r4[3][:], r4[3][:], r4[5][:], ALU.add, ["r4_3", "r4_5"], ["r4_3"])
                tt("dve", gate[:].rearrange("p n (g e) -> p n g e", g=4), r4[1][:].unsqueeze(3).to_broadcast([128, NT, 4, 4]),
                   r4[3][:].unsqueeze(2).to_broadcast([128, NT, 4, 4]), ALU.mult, ["r4_1", "r4_3"], ["gate"])
                for n in range(NT):
                    b = 4 + (n // 4) % 2
                    P.op("pe", lambda e, n=n, b=b: e.transpose(ps[b][0:16, (n % 4) * 128:(n % 4 + 1) * 128], gate[:, n, :], ident[:]),
                         reads=["gate", "ident"], writes=[PSR[b]])
                    if n % 4 == 3:
                        gcols = slice((n // 4) * 512, (n // 4 + 1) * 512)
                        cp("act", gateT[0:16, gcols], ps[b][0:16, :], [PSR[b]], ["gateT"])
                        cp("dve", gTb[0:16, gcols], gateT[0:16, gcols], ["gateT"], ["gTb"])
                        tt("dve", gTb[32:48, gcols], gateT[0:16, gcols], gTb[0:16, gcols], ALU.subtract, ["gateT", "gTb"], ["gTb"])
                if "gate" in dbg_d:
                    P.dma("sp", dbg_d["gate"], gate[:], "dbg", reads=["gate"])
                if "lg" in dbg_d:
                    P.dma("sp", dbg_d["lg"], lg[:], "dbg", reads=["lg"])
                if "ohg" in dbg_d:
                    P.dma("sp", dbg_d["ohg"], r4[1][:], "dbg", reads=["r4_1"])
                    P.dma("sp", dbg_d["elsel"], r4[2][:], "dbg", reads=["r4_2"])
                    P.dma("sp", dbg_d["rg"], rg[:], "dbg", reads=["rg%d" % i for i in range(7)])
                for n in range(NT):
                    act(x_sb[:, n, :], x_sb[:, n, :], AF.Copy, [("x", n)], [("x", n)], scale=ALPHA)
                for G in range(4):
                    for ee in range(4):
                        e_ = G * 4 + ee
                        s_ = e_ % 2
                        P.dma("pool", wgu[s_][:, :, 0:256], wg_d[l, e_].rearrange("(k p) c -> p k c", p=128), "wgu%d" % s_, writes=[("wgu", s_)])
                        P.dma("pool", wgu[s_][:, :, 256:512], wu_d[l, e_].rearrange("(k p) c -> p k c", p=128), "wgu%d" % s_, writes=[("wgu", s_)])
                        P.dma("pool", wdn[ee][:], wd_d[l, e_].rearrange("(c p) d -> p c d", p=128), "wdn%d" % ee, writes=[("wdn", ee)])
                        for tb in range(NB):
                            tcols = slice(tb * 512, (tb + 1) * 512)
                            def proj(cb):
                                for k in range(8):
                                    mm(ps[cb][:], wgu[s_][:, k, cb * 128:(cb + 1) * 128], hT[:, k, tcols], k == 0, k == 7,
                                       [("wgu", s_), ("hT", tb)], [PSR[cb]])
                            pp = tb % 2
                            proj(0)
                            act(sl[0][pp], ps[0][:], AF.Silu, [PSR[0]], [("sl", 0, pp)])
                            mm(ps[4][:], sel[:, e_, :], gTb[:, tcols], True, True, ["sel", "gTb"], [PSR[4]])
                            cp("act", gbs[pp][:], ps[4][:], [PSR[4]], [("gbs", pp)])
                            proj(1)
                            act(sl[1][pp], ps[1][:], AF.Silu, [PSR[1]], [("sl", 1, pp)])
                            for fc in range(2):
                                proj(2 + fc)
                                tt("dve", sl[fc][pp], sl[fc][pp], ps[2 + fc][:], ALU.mult, [("sl", fc, pp), PSR[2 + fc]], [("sl", fc, pp)])
                                tt("dve", hid[:, ee * 2 + fc, tcols], sl[fc][pp], gbs[pp][:], ALU.mult, [("sl", fc, pp), ("gbs", pp)], [("hid", tb)])
                    def stage_b(n):
                        for hf in range(2):
                            b = 5 + (2 * n + hf) % 3
                            for i8 in range(8):
                                mm(ps[b][:], hid[:, i8, n * 128:(n + 1) * 128], wdn[i8 // 2][:, i8 % 2, hf * 512:(hf + 1) * 512],
                                   i8 == 0, i8 == 7, [("hid", n // 4), ("wdn", i8 // 2)], [PSR[b]])
                            tt("dve", x_sb[:, n, hf * 512:(hf + 1) * 512], x_sb[:, n, hf * 512:(hf + 1) * 512], ps[b][:], ALU.add,
                               [("x", n), PSR[b]], [("x", n)])
                    if G < 3:
                        for n in range(NT):
                            stage_b(n)
                    else:
                        last = (l == NL - 1)

                        def ln2_a(n):
                            yield from layer_norm(x_sb[:, n, :], ("x", n), x_sb[:, n, :], ("x", n), lng[:], lnb[:], stp, n % 4, inplace=True)

                        def ln2_b(n):
                            if last:
                                P.dma("sp", out_d[n * 128:(n + 1) * 128, :], x_sb[:, n, :], "so%d" % (n % 2), reads=[("x", n)])
                            else:
                                P.dma("sp", x_scr[n * 128:(n + 1) * 128, :], x_sb[:, n, :], "so%d" % (n % 2), reads=[("x", n)])
                                transposes(n, [0, 1, 2, 3], x_sb[:, n, :], ("x", n))
                        for g4 in range(4):
                            for n in range(g4 * 4, g4 * 4 + 4):
                                stage_b(n)
                            if g4 >= 1:
                                for n in range((g4 - 1) * 4, g4 * 4):
                                    ln2_b(n)
                            interleave([ln2_a(n) for n in range(g4 * 4, g4 * 4 + 4)])
                        for n in range(12, 16):
                            ln2_b(n)
                P.flush()

        P.flush(final=True)
    return nc


def prep_shared(inp, S=2048):
    f = np.float32
    L = NLT
    G, Pn, N = 32, 64, 16
    out = {}
    out["w_in"] = np.ascontiguousarray(inp["w_in"], f)
    out["w_branch"] = np.ascontiguousarray(inp["w_branch"], f).reshape(L, 3 * 512, D)
    out["w_out"] = np.ascontiguousarray(inp["w_out"], f)
    lr, li, ldt = inp["ssm_lambda_re"], inp["ssm_lambda_im"], inp["ssm_log_dt"]
    ldt_full = np.broadcast_to(ldt[:, :, None], (L, G, Pn))
    stack = np.stack([lr, li, ldt_full], axis=-1).reshape(L, G * Pn, 3)
    out["lampq"] = np.ascontiguousarray(stack.reshape(L, 16, 128, 3).transpose(0, 2, 1, 3), f)
    rep = np.stack([lr.reshape(L, -1), li.reshape(L, -1), ldt_full.reshape(L, -1)], axis=1)
    out["lamrep"] = np.ascontiguousarray(np.broadcast_to(rep[:, None], (L, 128, 3, 2048)), f)
    def bblk(b):
        o = np.zeros((L, 128, 16, 128), f)
        for j in range(16):
            for g2 in range(2):
                g = 2 * j + g2
                gl = g % 8
                o[:, gl * 16:(gl + 1) * 16, j, g2 * 64:(g2 + 1) * 64] = b[:, g].transpose(0, 2, 1)
        return o
    def cblk(c):
        o = np.zeros((L, 128, 16, 128), f)
        for j in range(16):
            for g2 in range(2):
                g = 2 * j + g2
                gl = g % 8
                o[:, g2 * 64:(g2 + 1) * 64, j, gl * 16:(gl + 1) * 16] = c[:, g].transpose(0, 2, 1)
        return o
    out["bblk_re"] = bblk(inp["ssm_b_re"])
    out["bblk_im"] = bblk(inp["ssm_b_im"])
    out["bblkT_re"] = cblk(np.ascontiguousarray(inp["ssm_b_re"].transpose(0, 1, 3, 2)))
    out["bblkT_im"] = cblk(np.ascontiguousarray(inp["ssm_b_im"].transpose(0, 1, 3, 2)))
    out["cblk_re"] = cblk(inp["ssm_c_re"])
    out["cblk_im"] = cblk(inp["ssm_c_im"])
    out["dpq"] = np.ascontiguousarray(inp["ssm_d"].reshape(L, 4, 128).transpose(0, 2, 1), f)
    out["w_glu"] = np.ascontiguousarray(inp["ssm_w_glu"], f)
    out["dlam"] = np.ascontiguousarray(np.broadcast_to(inp["diff_lambda"][:, None], (L, 128, 4, 64)), f)
    out["dgrep"] = np.ascontiguousarray(np.broadcast_to(inp["diff_norm_g"][:, None], (L, 128, 512)), f)
    out["fb"] = np.ascontiguousarray(inp["fox_f_bias"].reshape(L, 8, 1), f)
    lnst = np.stack([inp["ln1_g"], inp["ln1_b"], inp["ln2_g"], inp["ln2_b"]], axis=1)
    out["lnrep"] = np.ascontiguousarray(np.broadcast_to(lnst[:, :, None], (L, 4, 128, D)), f)
    out["w_r"] = np.ascontiguousarray(np.concatenate([inp["moe_w_group"], inp["moe_w_expert"]], axis=-1), f)
    br = np.concatenate([inp["moe_b_group"], inp["moe_b_expert"]], axis=-1)
    out["b_rrep"] = np.ascontiguousarray(np.broadcast_to(br[:, None], (L, 128, 20)), f)
    out["moe_w_gate"] = np.ascontiguousarray(inp["moe_w_gate"], f)
    out["moe_w_up"] = np.ascontiguousarray(inp["moe_w_up"], f)
    out["moe_w_down"] = np.ascontiguousarray(inp["moe_w_down"], f)
    pos = np.arange(S, dtype=np.float32)
    inv_freq = (10000.0 ** (-np.arange(0, 64, 2, dtype=np.float32) / 64)).astype(np.float32)
    ang = pos[None, :] * inv_freq[np.arange(128) % 32][:, None]
    out["ropec"] = np.cos(ang).astype(f)
    out["ropes"] = np.sin(ang).astype(f)
    sel = np.zeros((128, 16, 128), f)
    for e in range(16):
        sel[e, e, :] = 1.0
        sel[32 + e, e, :] = 1.0
    out["sel"] = sel
    return out


_NC_CACHE = {}


def kernel(**inputs):
    S = 2048
    shared = prep_shared(inputs, S)
    x = np.ascontiguousarray(inputs["x"], np.float32)
    if "nc" not in _NC_CACHE:
        _NC_CACHE["nc"] = build(NL=4, S=S)
    nc = _NC_CACHE["nc"]
    in_maps = []
    for b in range(8):
        m = dict(shared)
        m["x"] = x[b]
        in_maps.append(m)
    res = run_bass_kernel_spmd(nc, in_maps, core_ids=list(range(8)))
    return np.stack([np.asarray(r["out"], np.float32) for r in res.results], axis=0)
```
